# Optimizing a Trainium2 kernel written in Bass

```python
import jax, jax.numpy as jnp
from jax import lax
import numpy as np

D_MODEL = 1024
BATCH = 8
SEQ = 4096
DEPTH = 1

RW_HEAD_DIM = 64
RW_WIDTH = D_MODEL // 2
RW_HEADS = RW_WIDTH // RW_HEAD_DIM
RW_DECAY_LORA = 64
RW_AAA_LORA = 64
RW_GATE_LORA = 128
RW_COLS = 3 * RW_WIDTH + RW_DECAY_LORA + RW_AAA_LORA + RW_GATE_LORA
RW_LNX_EPS = 64e-5
GM_WIDTH = D_MODEL // 2
GM_GROUP_DIM = 64
GM_GROUPS = GM_WIDTH // GM_GROUP_DIM
GM_CHUNK = 128
N_BRANCH = 2
IN_COLS = RW_COLS + 2 * GM_WIDTH + N_BRANCH * D_MODEL
N_EXPERTS = 256
TOP_K = 8
EXPERT_DIM = D_MODEL // 4
SHARED_DIM = D_MODEL // 4
ROUTED_SCALE = 2.5
MOE_BLOCK = 128
NORM_EPS = 1e-6
LN_EPS = 1e-5

kernel_name = 'hybrid_rwkv7_gmlp_moe_adaln'


def rmsnorm(x, g):
    xf = x.astype(jnp.float32)
    y = xf * lax.rsqrt(jnp.mean(xf * xf, -1, keepdims=True) + NORM_EPS)
    return (y * g.astype(jnp.float32)).astype(x.dtype)


def token_shift(p, mu):
    prev = jnp.pad(p, ((0, 0), (1, 0), (0, 0)))[:, :-1]
    return p + (prev - p) * mu


def wkv7_scan(r, w, k, v, a, b):
    Bb, Ss, H, N = r.shape

    def step(st, inp):
        r_t, w_t, k_t, v_t, a_t, b_t = inp
        sa = jnp.einsum('bhij,bhj->bhi', st, a_t)
        st = st * w_t[:, :, None, :] + sa[..., None] * b_t[:, :, None, :] + v_t[..., None] * k_t[:, :, None, :]
        return st, jnp.einsum('bhij,bhj->bhi', st, r_t)

    xs = tuple(jnp.moveaxis(t, 1, 0) for t in (r, w, k, v, a, b))
    _, ys = lax.scan(step, jnp.zeros((Bb, H, N, N), jnp.float32), xs)
    return jnp.moveaxis(ys, 0, 1)


def rwkv7_branch(p_rw, mu, w0, w2, a0, a2, g2, k_k, k_a, r_k, lnx_g, lnx_b):
    f32 = jnp.float32
    Bb, Ss, _ = p_rw.shape
    C = RW_WIDTH
    xs = token_shift(p_rw.astype(f32), mu)
    r = xs[..., :C]
    k = xs[..., C:2 * C]
    v = xs[..., 2 * C:3 * C]
    o = 3 * C
    xw = xs[..., o:o + RW_DECAY_LORA]
    o += RW_DECAY_LORA
    xa = xs[..., o:o + RW_AAA_LORA]
    o += RW_AAA_LORA
    xg = xs[..., o:o + RW_GATE_LORA]
    w_log = -jax.nn.softplus(-(w0 + jnp.tanh(xw) @ w2)) - 0.5
    decay = jnp.exp(-jnp.exp(w_log))
    a = jax.nn.sigmoid(a0 + xa @ a2)
    g = jax.nn.sigmoid(xg) @ g2

    def heads(t):
        return t.reshape(Bb, Ss, RW_HEADS, RW_HEAD_DIM)

    kk = heads(k * k_k)
    kk = kk * lax.rsqrt(jnp.maximum(jnp.sum(kk * kk, -1, keepdims=True), 1e-24))
    k = k * (1.0 + (a - 1.0) * k_a)
    rh, kh, vh, ah = heads(r), heads(k), heads(v), heads(a)
    y = wkv7_scan(rh, heads(decay), kh, vh, -kk, kk * ah)
    m = jnp.mean(y, -1, keepdims=True)
    var = jnp.mean(jnp.square(y - m), -1, keepdims=True)
    y = (y - m) * lax.rsqrt(var + RW_LNX_EPS) * lnx_g.reshape(RW_HEADS, RW_HEAD_DIM) \
        + lnx_b.reshape(RW_HEADS, RW_HEAD_DIM)
    y = y + jnp.sum(rh * kh * r_k, -1, keepdims=True) * vh
    return y.reshape(Bb, Ss, C) * g


def gmlp_branch(p_gm, ln_g, ln_b, w_s, b_s):
    f32 = jnp.float32
    Bb, Ss, _ = p_gm.shape
    nC = Ss // GM_CHUNK
    z = jax.nn.gelu(p_gm.astype(f32))
    u = z[..., :GM_WIDTH].reshape(Bb, nC, GM_CHUNK, GM_GROUPS, GM_GROUP_DIM)
    v = z[..., GM_WIDTH:].reshape(Bb, nC, GM_CHUNK, GM_GROUPS, GM_GROUP_DIM)
    m = jnp.mean(v, -1, keepdims=True)
    var = jnp.mean(jnp.square(v - m), -1, keepdims=True)
    vn = (v - m) * lax.rsqrt(var + LN_EPS) * ln_g + ln_b
    ws = w_s * jnp.tril(jnp.ones((GM_CHUNK, GM_CHUNK), f32))
    sv = jnp.einsum('gts,bnsgc->bntgc', ws, vn) + jnp.swapaxes(b_s, 0, 1)[:, :, None]
    return (u * sv).reshape(Bb, Ss, GM_WIDTH)


def swiglu(x, wg, wu, wd):
    return (jax.nn.silu(x @ wg) * (x @ wu)) @ wd


def moe(h, router_w, router_b, w_gate, w_up, w_down, s_gate, s_up, s_down):
    f32 = jnp.float32
    Bb, Ss, D = h.shape
    N = Bb * Ss
    NK = N * TOP_K
    hf = h.reshape(N, D)
    scores = jax.nn.sigmoid((hf @ router_w).astype(f32))
    _, idx = lax.top_k(scores + router_b.astype(f32), TOP_K)
    sel = jnp.take_along_axis(scores, idx, axis=1)
    wts = sel / jnp.sum(sel, -1, keepdims=True) * ROUTED_SCALE
    flat_e = idx.reshape(-1)
    flat_tok = jnp.arange(NK, dtype=jnp.int32) // TOP_K
    flat_w = wts.reshape(-1)
    order = jnp.argsort(flat_e)
    se = flat_e[order]
    counts = jnp.bincount(flat_e, length=N_EXPERTS)
    starts = jnp.cumsum(counts) - counts
    padded = (counts + MOE_BLOCK - 1) // MOE_BLOCK * MOE_BLOCK
    pends = jnp.cumsum(padded)
    pstarts = pends - padded
    dest = pstarts[se] + (jnp.arange(NK, dtype=jnp.int32) - starts[se])
    P = NK + N_EXPERTS * MOE_BLOCK
    nb = P // MOE_BLOCK
    row_tok = jnp.full((P,), N, jnp.int32).at[dest].set(flat_tok[order])
    row_w = jnp.zeros((P,), f32).at[dest].set(flat_w[order])
    block_e = jnp.minimum(jnp.searchsorted(pends, jnp.arange(nb, dtype=jnp.int32) * MOE_BLOCK, side='right'), N_EXPERTS - 1)
    hpad = jnp.concatenate([hf, jnp.zeros((1, D), hf.dtype)], axis=0)

    def body(y, blk):
        tok, wt, e = blk
        xb = hpad[tok]
        ob = swiglu(xb, w_gate[e], w_up[e], w_down[e]) * wt[:, None].astype(xb.dtype)
        return y.at[tok].add(ob.astype(y.dtype)), None

    y, _ = lax.scan(body, jnp.zeros((N + 1, D), hf.dtype),
                    (row_tok.reshape(nb, MOE_BLOCK), row_w.reshape(nb, MOE_BLOCK), block_e))
    out = y[:N] + swiglu(hf, s_gate, s_up, s_down)
    return out.reshape(Bb, Ss, D)


def setup_inputs(seed: int = 0) -> dict:
    key = jax.random.key(seed)
    ks = iter(jax.random.split(key, 48))
    f32 = jnp.float32
    L = DEPTH
    D = D_MODEL

    def nrm(shape, scale):
        return jax.random.normal(next(ks), shape, f32) * scale

    def near_one(shape):
        return 1.0 + 0.1 * jax.random.normal(next(ks), shape, f32)

    return {
        'x': nrm((BATCH, SEQ, D), 1.0),
        'c': nrm((BATCH, D), 1.0),
        'w_ada': nrm((L, D, 6 * D), 0.5 * D ** -0.5),
        'b_ada': nrm((L, 6 * D), 0.02),
        'norm1_g': near_one((L, D)),
        'w_in': nrm((L, D, IN_COLS), D ** -0.5),
        'rw_mu': jax.random.uniform(next(ks), (L, RW_COLS), f32),
        'rw_w0': nrm((L, RW_WIDTH), 0.5) - 0.5,
        'rw_w2': nrm((L, RW_DECAY_LORA, RW_WIDTH), 0.5 * RW_DECAY_LORA ** -0.5),
        'rw_a0': nrm((L, RW_WIDTH), 0.5),
        'rw_a2': nrm((L, RW_AAA_LORA, RW_WIDTH), 0.5 * RW_AAA_LORA ** -0.5),
        'rw_g2': nrm((L, RW_GATE_LORA, RW_WIDTH), RW_GATE_LORA ** -0.5),
        'rw_k_k': near_one((L, RW_WIDTH)),
        'rw_k_a': near_one((L, RW_WIDTH)),
        'rw_r_k': nrm((L, RW_HEADS, RW_HEAD_DIM), 0.1),
        'rw_lnx_g': near_one((L, RW_WIDTH)),
        'rw_lnx_b': nrm((L, RW_WIDTH), 0.02),
        'gm_ln_g': near_one((L, GM_GROUPS, GM_GROUP_DIM)),
        'gm_ln_b': nrm((L, GM_GROUPS, GM_GROUP_DIM), 0.02),
        'gm_w_s': nrm((L, GM_GROUPS, GM_CHUNK, GM_CHUNK), GM_CHUNK ** -0.5),
        'gm_b_s': near_one((L, GM_GROUPS, GM_CHUNK)),
        'w_br_rwkv': nrm((L, RW_WIDTH, D), RW_WIDTH ** -0.5),
        'w_br_gmlp': nrm((L, GM_WIDTH, D), GM_WIDTH ** -0.5),
        'w_out': nrm((L, D, D), D ** -0.5),
        'norm2_g': near_one((L, D)),
        'router_w': nrm((L, D, N_EXPERTS), D ** -0.5),
        'router_b': nrm((L, N_EXPERTS), 0.01),
        'moe_w_gate': nrm((L, N_EXPERTS, D, EXPERT_DIM), D ** -0.5),
        'moe_w_up': nrm((L, N_EXPERTS, D, EXPERT_DIM), D ** -0.5),
        'moe_w_down': nrm((L, N_EXPERTS, EXPERT_DIM, D), EXPERT_DIM ** -0.5),
        'sh_w_gate': nrm((L, D, SHARED_DIM), D ** -0.5),
        'sh_w_up': nrm((L, D, SHARED_DIM), D ** -0.5),
        'sh_w_down': nrm((L, SHARED_DIM, D), SHARED_DIM ** -0.5),
        'final_g': near_one((D,)),
    }


def reference(x, c, w_ada, b_ada, norm1_g, w_in, rw_mu, rw_w0, rw_w2, rw_a0, rw_a2, rw_g2,
              rw_k_k, rw_k_a, rw_r_k, rw_lnx_g, rw_lnx_b, gm_ln_g, gm_ln_b, gm_w_s, gm_b_s,
              w_br_rwkv, w_br_gmlp, w_out, norm2_g, router_w, router_b, moe_w_gate, moe_w_up,
              moe_w_down, sh_w_gate, sh_w_up, sh_w_down, final_g):
    Bb, Ss, D = x.shape
    for l in range(DEPTH):
        mod = (jax.nn.silu(c) @ w_ada[l] + b_ada[l]).reshape(Bb, 6, D)
        sh1, sc1, gt1 = mod[:, 0, None, :], mod[:, 1, None, :], mod[:, 2, None, :]
        sh2, sc2, gt2 = mod[:, 3, None, :], mod[:, 4, None, :], mod[:, 5, None, :]

        h = rmsnorm(x, norm1_g[l]) * (1.0 + sc1) + sh1
        p = h @ w_in[l]
        p_rw = p[..., :RW_COLS]
        p_gm = p[..., RW_COLS:RW_COLS + 2 * GM_WIDTH]
        p_gt = p[..., RW_COLS + 2 * GM_WIDTH:]
        o_rw = rwkv7_branch(p_rw, rw_mu[l], rw_w0[l], rw_w2[l], rw_a0[l], rw_a2[l], rw_g2[l],
                            rw_k_k[l], rw_k_a[l], rw_r_k[l], rw_lnx_g[l], rw_lnx_b[l]).astype(x.dtype)
        o_gm = gmlp_branch(p_gm, gm_ln_g[l], gm_ln_b[l], gm_w_s[l], gm_b_s[l]).astype(x.dtype)
        gates = jax.nn.sigmoid(p_gt.astype(jnp.float32)).astype(x.dtype)
        merged = gates[..., :D] * (o_rw @ w_br_rwkv[l]) + gates[..., D:] * (o_gm @ w_br_gmlp[l])
        x = x + gt1 * (merged @ w_out[l])

        h2 = rmsnorm(x, norm2_g[l]) * (1.0 + sc2) + sh2
        x = x + gt2 * moe(h2, router_w[l], router_b[l], moe_w_gate[l], moe_w_up[l], moe_w_down[l],
                          sh_w_gate[l], sh_w_up[l], sh_w_down[l])
    return rmsnorm(x, final_g)
```

```python
import contextlib
import numpy as np
import concourse.bass as bass
import concourse.mybir as mybir
from concourse.bass_utils import run_bass_kernel_spmd

F32 = mybir.dt.float32
BF16 = mybir.dt.bfloat16
I32 = mybir.dt.int32
AF = mybir.ActivationFunctionType
ALU = mybir.AluOpType
AX = mybir.AxisListType

D = 1024
SEQ = 4096
NE = 256
INC = 4864
RWC = 1792


class Buf:
    __slots__ = ("name", "lw", "rd")

    def __init__(self, name=""):
        self.name = name
        self.lw = None
        self.rd = []


class Op:
    __slots__ = ("eng", "fn", "deps", "needed", "dma", "tok", "sem", "line")

    def __init__(self, eng, fn, dma):
        self.eng = eng
        self.fn = fn
        self.deps = []
        self.needed = False
        self.dma = dma
        self.tok = None
        self.sem = None


class Prog:
    ENGS = ("sync", "scalar", "vector", "gpsimd", "tensor")

    def __init__(self, n_dma_sems=32):
        self.ops = []
        self.n_dma = n_dma_sems
        self.dma_rr = 0
        self.dma_last = [None] * n_dma_sems
        self.dma_cnt = [0] * n_dma_sems
        self.last_eng = {}
        self.bar = []
        self.gen = 0
        self.eng_gen = {}

    def barrier(self):
        lasts = [self.last_eng[e] for e in self.ENGS if e in self.last_eng]
        lasts += [d for d in self.dma_last if d is not None]
        self.bar = lasts
        self.gen += 1

    def add(self, eng, fn, reads=(), writes=(), dma=False):
        op = Op(eng, fn, dma)
        import sys as _s
        op.line = _s._getframe(2).f_lineno
        deps = []
        for b in reads:
            if b.lw is not None:
                deps.append(b.lw)
        for b in writes:
            if b.lw is not None:
                deps.append(b.lw)
            deps.extend(b.rd)
        if dma:
            j = self.dma_rr
            self.dma_rr = (self.dma_rr + 1) % self.n_dma
            if self.dma_last[j] is not None:
                deps.append(self.dma_last[j])
            self.dma_last[j] = op
            self.dma_cnt[j] += 1
            op.sem = ("dma", j)
            op.tok = 16 * self.dma_cnt[j]
            op.needed = True
        else:
            op.sem = ("eng", eng)
        if self.eng_gen.get(eng, 0) < self.gen:
            deps.extend(self.bar)
            self.eng_gen[eng] = self.gen
        if not dma:
            self.last_eng[eng] = op
        seen = set()
        for d in deps:
            if id(d) in seen or d is op:
                continue
            seen.add(id(d))
            if (not d.dma) and (not dma) and d.eng == eng and eng == "tensor":
                continue
            op.deps.append(d)
            d.needed = True
        for b in reads:
            if dma:
                b.rd.append(op)
            else:
                b.rd = [r for r in b.rd if r.dma or r.eng != eng]
                b.rd.append(op)
        for b in writes:
            b.lw = op
            b.rd = []
        self.ops.append(op)
        return op

    def emit(self, nc):
        import os
        if os.environ.get("TRUNC"):
            self.ops = self.ops[:int(os.environ["TRUNC"])]
            self.dma_cnt = [0] * self.n_dma
            for op in self.ops:
                if op.dma:
                    self.dma_cnt[op.sem[1]] += 1
        print("n_ops", len(self.ops), flush=True)
        cnt = {e: 0 for e in self.ENGS}
        for op in self.ops:
            if not op.dma and op.needed:
                cnt[op.eng] += 1
                op.tok = cnt[op.eng]
        with contextlib.ExitStack() as st:
            esem = {e: st.enter_context(nc.semaphore("p_" + e)) for e in self.ENGS}
            dsem = [st.enter_context(nc.semaphore("d_%d" % j)) for j in range(self.n_dma)]
            block = st.enter_context(nc.Block())

            def semof(op):
                return dsem[op.sem[1]] if op.sem[0] == "dma" else esem[op.sem[1]]

            def run(engname):
                def body(eng):
                    waited = {}
                    for op in self.ops:
                        if op.eng != engname:
                            continue
                        for d in op.deps:
                            if waited.get(d.sem, 0) >= d.tok:
                                continue
                            waited[d.sem] = d.tok
                            eng.wait_ge(semof(d), d.tok)
                        inst = op.fn(eng)
                        if op.dma:
                            inst.then_inc(semof(op), 16)
                        elif op.needed:
                            inst.then_inc(semof(op), 1)
                    if engname == "sync":
                        for j in range(self.n_dma):
                            if self.dma_cnt[j]:
                                eng.wait_ge(dsem[j], 16 * self.dma_cnt[j])
                return body

            block.sync(run("sync"))
            block.scalar(run("scalar"))
            block.vector(run("vector"))
            block.gpsimd(run("gpsimd"))
            block.tensor(run("tensor"))


class Tl:
    def __init__(self, t, name):
        self.t = t
        self.b = Buf(name)

    def __getitem__(self, k):
        return self.t[k]


class Arena:
    def __init__(self, nc, st, name, nbytes):
        self.t = st.enter_context(nc.sbuf_tensor(name, [128, nbytes // 2], BF16))
        self.cap = nbytes // 2
        self.off = 0
        self.pers = 0
        self.name = name

    def reset(self):
        self.off = self.pers

    def alloc(self, name, shape, dt=F32, pers=False):
        esz = 2 if dt == BF16 else 4
        n = 1
        for s in shape[1:]:
            n *= s
        ne = n * esz // 2
        ne += ne % 2
        if pers:
            assert self.off == self.pers, "persistent alloc only right after reset"
        assert self.off + ne <= self.cap, (self.name, name, self.off, ne, self.cap)
        ap = self.t[:, self.off:self.off + n * esz // 2]
        self.off += ne
        if pers:
            self.pers = self.off
        if esz == 4:
            ap = ap.bitcast(dt)
        if len(shape) == 3:
            ap = ap.rearrange("p (a b) -> p a b", a=shape[1])
        elif len(shape) == 4:
            ap = ap.rearrange("p (a b c) -> p a b c", a=shape[1], b=shape[2])
        return Tl(ap, name)


def build(S=SEQ, stage=9, dbg=False):
    NT = S // 128
    NBLK = NT * 8 + NE
    NQ = (NBLK + 127) // 128
    nc = bass.Bass("TRN2", target_bir_lowering=False)
    P = Prog()
    st = contextlib.ExitStack()

    def din(name, shape, dt=F32):
        return nc.dram_tensor(name, shape, dt, kind="ExternalInput").ap()

    x_d = din("x", [S, D])
    c_d = din("c_pk", [128, 8])
    wada_d = din("w_ada", [D, 6 * D])
    bada_d = din("b_ada", [6 * D])
    n1g_d = din("norm1_g", [D])
    n2g_d = din("norm2_g", [D])
    fing_d = din("final_g", [D])
    win_d = din("w_in", [D, INC])
    mu_d = din("mu_fm", [128, 14])
    pf_d = din("pf", [128, 7, 4])
    w2_d = din("rw_w2", [64, 512])
    a2_d = din("rw_a2", [64, 512])
    g2_d = din("rw_g2", [128, 512])
    lng_d = din("gm_ln_g", [512])
    lnb_d = din("gm_ln_b", [512])
    wsT_d = din("wsT", [128, 8, 128])
    bs_d = din("bs_tm", [128, 8])
    wbr_rw_d = din("w_br_rwkv", [512, D])
    wbr_gm_d = din("w_br_gmlp", [512, D])
    wout_d = din("w_out", [D, D])
    rw_d = din("router_w", [D, NE])
    rb_d = din("router_b", [NE])
    MR = NE * 128 if stage >= 3 else 128
    mg_d = din("moe_wg", [MR, 2048])
    mup_d = din("moe_wu", [MR, 2048])
    md_d = din("moe_wd", [MR, 2048])
    sg_d = din("sh_w_gate", [D, 256])
    su_d = din("sh_w_up", [D, 256])
    sd_d = din("sh_w_down", [256, D])
    cst_d = din("cst", [128, 5, 128])
    misc_d = din("misc", [128, 4])
    iota_d = din("iota1", [NE])
    out_d = nc.dram_tensor("out", [S, D], F32, kind="ExternalOutput").ap()

    def dscr(name, shape, dt):
        return nc.dram_tensor(name, shape, dt, kind="Internal").ap()

    hT_d = dscr("hT_s", [NT, 128, 8 * 128], BF16); hT_db = Buf()
    orw_d = dscr("orw_s", [NT, 128, 512], BF16); orw_db = Buf()
    ogT_d = dscr("ogT_s", [NT, 128, 512], BF16); ogT_db = Buf()
    acc_d = dscr("acc_s", [S, D], F32); acc_db = Buf()
    h2_d = dscr("h2_s", [S, D], BF16); h2_db = Buf()
    xs_d = dscr("xs_s", [NBLK * 128, D], BF16); xs_db = Buf()
    ob_d = dscr("ob_s", [NBLK * 128, D], F32); ob_db = Buf()
    outb = Buf()
    dbg_d = nc.dram_tensor("dbg", [S, D], F32, kind="ExternalOutput").ap() if dbg else None
    dbgb = Buf()

    PH = Arena(nc, st, "arena", 207 * 1024)

    class _Pers:
        def alloc(self, name, shape, dt=F32):
            return PH.alloc(name, shape, dt, pers=True)
    PERS = _Pers()

    def DMA(eng, out, in_, R, W):
        P.add(eng, lambda e: e.dma_start(out=out, in_=in_), reads=R, writes=W, dma=True)

    def MM(out, lhsT, rhs, R, W, start=True, stop=True):
        P.add("tensor", lambda e: e.matmul(out, lhsT=lhsT, rhs=rhs, start=start, stop=stop), reads=R, writes=W)

    def TR(out, in_, ident, R, W):
        P.add("tensor", lambda e: e.transpose(out, in_, ident), reads=R, writes=W)

    def ACT(out, in_, func, R, W, **kw):
        P.add("scalar", lambda e: e.activation(out=out, in_=in_, func=func, **kw), reads=R, writes=W)

    def TT(eng, out, in0, in1, op, R, W):
        P.add(eng, lambda e: e.tensor_tensor(out=out, in0=in0, in1=in1, op=op), reads=R, writes=W)

    def TS(eng, out, in0, s1, s2, op0, op1, R, W):
        if op1 is None:
            P.add(eng, lambda e: e.tensor_scalar(out=out, in0=in0, scalar1=s1, scalar2=None, op0=op0), reads=R, writes=W)
        else:
            P.add(eng, lambda e: e.tensor_scalar(out=out, in0=in0, scalar1=s1, scalar2=s2, op0=op0, op1=op1), reads=R, writes=W)

    def STT(eng, out, in0, scalar, in1, op0, op1, R, W):
        P.add("vector", lambda e: e.scalar_tensor_tensor(out=out, in0=in0, scalar=scalar, in1=in1, op0=op0, op1=op1), reads=R, writes=W)

    def CP(eng, out, in_, R, W):
        if eng == "scalar":
            P.add(eng, lambda e: e.copy(out=out, in_=in_), reads=R, writes=W)
        else:
            P.add(eng, lambda e: e.tensor_copy(out=out, in_=in_), reads=R, writes=W)

    def RSUM(eng, out, in_, R, W):
        P.add(eng, lambda e: e.reduce_sum(out=out, in_=in_, axis=AX.X), reads=R, writes=W)

    def MAX8(out, in_, R, W):
        P.add("vector", lambda e: e.max(out=out, in_=in_), reads=R, writes=W)

    def MSET(eng, ap, val, W):
        P.add(eng, lambda e: e.memset(ap, val), writes=W)

    def barrier():
        P.barrier()

    ps = st.enter_context(nc.psum_tensor("ps", [128, 8, 512], F32))
    psb = [Buf("psb%d" % i) for i in range(8)]
    rot = [0]

    def bank():
        i = rot[0]
        rot[0] = (i + 1) % 8
        return i

    def pf32(i):
        return ps[:, i, :]

    def pbf(i):
        return ps[:, i, :].bitcast(BF16)

    def RSQ(out, in_, buf):
        ACT(out, in_, AF.Sqrt, [buf], [buf])
        P.add("vector", lambda e: e.reciprocal(out=out, in_=out), reads=[buf], writes=[buf])

    def rmsnorm_rstd(src, sqf, ss, eps=1e-6):
        ACT(sqf[:], src[:], AF.Square, [src.b], [sqf.b])
        RSUM("vector", ss[:, 0:1], sqf[:], [sqf.b], [ss.b])
        TS("vector", ss[:, 1:2], ss[:, 0:1], 1.0 / D, eps, ALU.mult, ALU.add, [ss.b], [ss.b])
        RSQ(ss[:, 2:3], ss[:, 1:2], ss.b)

    def transpose8(src_bf, dst, n=8):
        bk = bank()
        for kc in range(n):
            TR(pbf(bk)[:, kc * 128:(kc + 1) * 128], src_bf[:, kc * 128:(kc + 1) * 128], ident_b, [src_bf.b, cst_b.b], [psb[bk]])
        CP("scalar", dst[:].rearrange("p a b -> p (a b)"), pbf(bk)[:, 0:n * 128], [psb[bk]], [dst.b])

    cst_f = PERS.alloc("cst_f", [128, 5, 128])
    cst_b = PERS.alloc("cst_b", [128, 5, 128], BF16)
    DMA("sync", cst_f[:], cst_d, [], [cst_f.b])
    DMA("gpsimd", cst_b[:], cst_d, [], [cst_b.b])
    ident_b = cst_b[:, 0, :]
    blk_f = cst_f[:, 4, :]
    blk_b = cst_b[:, 4, :]
    ones_f = PERS.alloc("ones_f", [128, 256])
    MSET("vector", ones_f[:], 1.0, [ones_f.b])
    ones_b = PERS.alloc("ones_b", [128, 128], BF16)
    MSET("vector", ones_b[:], 1.0, [ones_b.b])
    misc = PERS.alloc("misc", [128, 4])
    DMA("sync", misc[:], misc_d, [], [misc.b])
    modr = PERS.alloc("modr", [128, 6, D])
    gs1 = PERS.alloc("gs1", [128, D])
    gs2 = PERS.alloc("gs2", [128, D])

    ct = PH.alloc("ct", [128, 8])
    sct = PH.alloc("sct", [128, 8])
    screp = PH.alloc("screp", [128, 8, 128])
    wst = [PH.alloc("wada%d" % i, [128, 8, 512]) for i in range(2)]
    ng = PH.alloc("ng", [128, 2, D])
    DMA("sync", ct[:], c_d, [], [ct.b])
    DMA("sync", modr[:].rearrange("p a d -> p (a d)"), bada_d.partition_broadcast(128), [], [modr.b])
    DMA("sync", ng[:, 0, :], n1g_d.partition_broadcast(128), [], [ng.b])
    DMA("sync", ng[:, 1, :], n2g_d.partition_broadcast(128), [], [ng.b])
    ACT(sct[:], ct[:], AF.Silu, [ct.b], [sct.b])
    for kc in range(8):
        ACT(screp[:, kc, :], ones_f[:, 0:128], AF.Copy, [ones_f.b, sct.b], [screp.b], scale=sct[:, kc:kc + 1])
    wv = wada_d.rearrange("(kc p) n -> p kc n", p=128)
    for j in range(12):
        w = wst[j % 2]
        DMA("sync", w[:], wv[:, :, j * 512:(j + 1) * 512], [], [w.b])
        bk = bank()
        for kc in range(8):
            MM(pf32(bk), screp[:, kc, :], w[:, kc, :], [screp.b, w.b], [psb[bk]], start=(kc == 0), stop=(kc == 7))
        a, hlf = j // 2, j % 2
        TT("vector", modr[:, a, hlf * 512:(hlf + 1) * 512], pf32(bk), modr[:, a, hlf * 512:(hlf + 1) * 512], ALU.add,
           [psb[bk], modr.b], [modr.b])
    STT("vector", gs1[:], modr[:, 1, :], 1.0, ng[:, 0, :], ALU.add, ALU.mult, [modr.b, ng.b], [gs1.b])
    STT("vector", gs2[:], modr[:, 4, :], 1.0, ng[:, 1, :], ALU.add, ALU.mult, [modr.b, ng.b], [gs2.b])
    sh1, gt1, sh2, gt2 = modr[:, 0, :], modr[:, 2, :], modr[:, 3, :], modr[:, 5, :]

    barrier()
    PH.reset()
    winA = PH.alloc("winA", [128, 8, 2816], BF16)
    winv = win_d.rearrange("(kc p) n -> p kc n", p=128)
    for kc in range(8):
        DMA("gpsimd", winA[:, kc, :], winv[:, kc, 0:2816], [], [winA.b])
    w2_b = PH.alloc("w2_b", [128, 512], BF16)
    a2_b = PH.alloc("a2_b", [128, 512], BF16)
    g2_b = PH.alloc("g2_b", [128, 512], BF16)
    DMA("gpsimd", w2_b[0:64, :], w2_d, [], [w2_b.b])
    DMA("gpsimd", a2_b[64:128, :], a2_d, [], [a2_b.b])
    DMA("gpsimd", g2_b[:], g2_d, [], [g2_b.b])
    wsT_b = PH.alloc("wsT_b", [128, 8, 128], BF16)
    DMA("gpsimd", wsT_b[:], wsT_d, [], [wsT_b.b])
    TT("gpsimd", wsT_b[:], wsT_b[:], cst_b[:, 2:3, :].to_broadcast([128, 8, 128]), ALU.mult, [wsT_b.b, cst_b.b], [wsT_b.b])
    mu = PH.alloc("mu", [128, 14])
    pfm = PH.alloc("pfm", [128, 7, 4])
    omka = PH.alloc("omka", [128, 4])
    DMA("sync", mu[:], mu_d, [], [mu.b])
    DMA("sync", pfm[:], pf_d, [], [pfm.b])
    TS("vector", omka[:], pfm[:, 3, :], -1.0, 1.0, ALU.mult, ALU.add, [pfm.b], [omka.b])
    lng = PH.alloc("lng", [128, 512])
    lnb = PH.alloc("lnb", [128, 512])
    bs_tm = PH.alloc("bs_tm", [128, 8])
    DMA("sync", lng[:], lng_d.partition_broadcast(128), [], [lng.b])
    DMA("sync", lnb[:], lnb_d.partition_broadcast(128), [], [lnb.b])
    DMA("sync", bs_tm[:], bs_d, [], [bs_tm.b])
    XTs = PH.alloc("xt", [128, D])
    sqf = Tl(modr[:, 1, :], "sqf")
    y_f = Tl(modr[:, 4, 0:512].rearrange("p (a b) -> p a b", a=4), "y_f")
    BVx = Tl(modr[:, 4, 512:1024].rearrange("p (a b) -> p a b", a=4), "BVx")
    ssn = PH.alloc("ssn", [128, 4])
    h_tm = PH.alloc("h_tm", [128, D], BF16)
    hT = PH.alloc("hT", [128, 8, 128], BF16)
    Praw = PH.alloc("Praw", [128, 14, 129])
    XS = PH.alloc("XS", [128, 14, 128])
    MSET("gpsimd", Praw[:, :, 0:1], 0.0, [Praw.b])
    tw = PH.alloc("tw", [128, 128], BF16)
    xa_b = PH.alloc("xa_b", [128, 128], BF16)
    sg_b = PH.alloc("sg_b", [128, 128], BF16)

    def F4(name):
        return PH.alloc(name, [128, 4, 128])

    def B4(name):
        return PH.alloc(name, [128, 4, 128], BF16)
    sgz, cum, Pt, iP, Pm1, alr, KK, rn, tmpk, kmod = [F4(n) for n in
        ("sgz", "cum", "Pt", "iP", "Pm1", "alr", "KK", "rn", "tmpk", "kmod")]
    sq_b, Bt, Kt, vb, orw_b = [B4(n) for n in ("sq_b", "Bt", "Kt", "vb", "orw_b")]
    rkb = sq_b
    IF = []
    for i in range(2):
        IF.append(dict(
            AR=PH.alloc("AR%d" % i, [128, 4, 2, 128], BF16), Vtm=PH.alloc("Vtm%d" % i, [128, 4, 128], BF16),
            BtT=PH.alloc("BtT%d" % i, [128, 4, 128], BF16), KtT=PH.alloc("KtT%d" % i, [128, 4, 128], BF16),
            XA=PH.alloc("XA%d" % i, [128, 8, 2, 128], BF16), KA2=PH.alloc("KA2%d" % i, [128, 8, 2, 128], BF16),
            XT0=PH.alloc("XT0%d" % i, [128, 8, 128], BF16), g_f=F4("g_f%d" % i), PL=PH.alloc("PL%d" % i, [128, 4]),
            BV=(F4("BV0") if i == 0 else BVx)))
    Xp = [PH.alloc("Xp%d" % i, [128, 8, 128], BF16) for i in range(2)]
    XTp = [PH.alloc("XTp%d" % i, [128, 8, 128], BF16) for i in range(2)]
    Mf = PH.alloc("Mf", [128, 8, 128])
    Mb = PH.alloc("Mb", [128, 8, 128], BF16)
    for _t in Xp + XTp + [Mf, Mb]:
        _t.bq = [Buf(_t.b.name + "q0"), Buf(_t.b.name + "q1")]
    yd = Tl(Mf[:, 0:4, :], "yd")
    yd.b = Mf.bq[0]
    yd2 = Tl(Mf[:, 4:8, :], "yd2")
    yd2.b = Mf.bq[1]
    W0T_b = PH.alloc("W0T_b", [128, 512], BF16)
    UT_b = PH.alloc("UT_b", [128, 512], BF16)
    ST_f = PH.alloc("ST_f", [128, 4, 64])
    ST_b = PH.alloc("ST_b", [128, 4, 64], BF16)
    MSET("gpsimd", ST_f[:], 0.0, [ST_f.b])
    MSET("gpsimd", ST_b[:], 0.0, [ST_b.b])
    ZU = [PH.alloc("zu%d" % i, [128, 512]) for i in range(2)]
    ZV = [PH.alloc("zv%d" % i, [128, 8, 64]) for i in range(2)]
    dv = PH.alloc("dv", [128, 8, 64])
    dv2 = PH.alloc("dv2", [128, 8, 64])
    gst = PH.alloc("gst", [128, 4, 8])
    vn_b = PH.alloc("vn_b", [128, 512], BF16)
    ogm_b = vn_b
    ogT_b = PH.alloc("ogT_b", [128, 4, 128], BF16)
    msk_2 = cst_f[:, 1:3, :]
    msk_l = cst_f[:, 3, :]

    EXPC = 0.6065306597126334
    DMA("sync", XTs[:], x_d[0:128, :], [], [XTs.b])

    def front(ti):
        zu, zv = ZU[ti % 2], ZV[ti % 2]
        xt = XTs
        rmsnorm_rstd(xt, sqf, ssn)
        STT("vector", sqf[:], xt[:], ssn[:, 2:3], gs1[:], ALU.mult, ALU.mult, [xt.b, ssn.b, gs1.b, sqf.b], [sqf.b])
        if ti + 1 < NT:
            DMA("sync", XTs[:], x_d[(ti + 1) * 128:(ti + 2) * 128, :], [], [XTs.b])
        TT("gpsimd", h_tm[:], sqf[:], sh1, ALU.add, [sqf.b, modr.b], [h_tm.b])
        yield
        transpose8(h_tm, hT)
        DMA("sync", hT_d[ti], hT[:].rearrange("p a b -> p (a b)"), [hT.b], [hT_db])
        yield
        for lo, hi in ((0, 4), (4, 8), (8, 12), (12, 14)):
            bk = bank()
            for f in range(lo, hi):
                for kc in range(8):
                    MM(pf32(bk)[:, (f - lo) * 128:(f - lo + 1) * 128], winA[:, kc, f * 128:(f + 1) * 128], hT[:, kc, :],
                       [winA.b, hT.b], [psb[bk]], start=(kc == 0), stop=(kc == 7))
            CP("scalar", Praw[:, lo:hi, 1:129], pf32(bk)[:, 0:(hi - lo) * 128].rearrange("p (a b) -> p a b", a=hi - lo),
               [psb[bk]], [Praw.b])
            yield
        for half, dst in ((0, zu[:]), (1, zv[:].rearrange("p a b -> p (a b)"))):
            bk = bank()
            for kc in range(8):
                MM(pf32(bk), hT[:, kc, :], winA[:, kc, RWC + half * 512:RWC + (half + 1) * 512], [winA.b, hT.b], [psb[bk]],
                   start=(kc == 0), stop=(kc == 7))
            ACT(dst, pf32(bk), AF.Gelu_apprx_tanh, [psb[bk]], [zu.b if half == 0 else zv.b])
            yield

    def prep(ti):
        I = IF[ti % 2]
        AR, Vtm, BtT, KtT, XA, KA2, XT0, g_f, PL, BV = (I[k] for k in ("AR", "Vtm", "BtT", "KtT", "XA", "KA2", "XT0", "g_f", "PL", "BV"))
        TT("vector", XS[:], Praw[:, :, 0:128], Praw[:, :, 1:129], ALU.subtract, [Praw.b], [XS.b])
        for f in range(14):
            STT("vector", XS[:, f, :], XS[:, f, :], mu[:, f:f + 1], Praw[:, f, 1:129], ALU.mult, ALU.add,
                [XS.b, mu.b, Praw.b], [XS.b])
        CP("gpsimd", Praw[:, :, 0:1], Praw[:, :, 128:129], [Praw.b, XS.b], [Praw.b])
        yield
        ACT(tw[0:64, :], XS[0:64, 12, :], AF.Tanh, [XS.b], [tw.b])
        CP("scalar", xa_b[64:128, :], XS[64:128, 12, :], [XS.b], [xa_b.b])
        ACT(sg_b[:], XS[:, 13, :], AF.Sigmoid, [XS.b], [sg_b.b])
        yield
        bz, ba, bg = bank(), bank(), bank()
        for f in range(4):
            MM(pf32(bz)[:, f * 128:(f + 1) * 128], w2_b[0:64, f * 128:(f + 1) * 128], tw[0:64, :], [w2_b.b, tw.b], [psb[bz]])
        for f in range(4):
            MM(pf32(ba)[:, f * 128:(f + 1) * 128], a2_b[64:128, f * 128:(f + 1) * 128], xa_b[64:128, :], [a2_b.b, xa_b.b], [psb[ba]])
        for f in range(4):
            MM(pf32(bg)[:, f * 128:(f + 1) * 128], g2_b[:, f * 128:(f + 1) * 128], sg_b[:], [g2_b.b, sg_b.b], [psb[bg]])
        for f in range(4):
            ACT(sgz[:, f, :], pf32(bz)[:, f * 128:(f + 1) * 128], AF.Sigmoid, [psb[bz], pfm.b], [sgz.b], bias=pfm[:, 0, f:f + 1])
        for f in range(4):
            ACT(alr[:, f, :], pf32(ba)[:, f * 128:(f + 1) * 128], AF.Sigmoid, [psb[ba], pfm.b], [alr.b], bias=pfm[:, 1, f:f + 1])
        CP("vector", g_f[:].rearrange("p a b -> p (a b)"), pf32(bg), [psb[bg]], [g_f.b])
        yield
        for f in range(4):
            P.add("vector", (lambda o, d1: (lambda e: e.tensor_tensor_scan(out=o, data0=ones_f[:, 0:128], data1=d1, initial=0.0,
                                                                             op0=ALU.mult, op1=ALU.add)))(cum[:, f, :], sgz[:, f, :]),
                  reads=[sgz.b, ones_f.b], writes=[cum.b])
        ACT(Pt[:], cum[:], AF.Exp, [cum.b], [Pt.b], scale=-EXPC)
        ACT(iP[:], cum[:], AF.Exp, [cum.b], [iP.b], scale=EXPC)
        TT("gpsimd", tmpk[:], cum[:], sgz[:], ALU.subtract, [cum.b, sgz.b], [tmpk.b])
        ACT(Pm1[:], tmpk[:], AF.Exp, [tmpk.b], [Pm1.b], scale=-EXPC)
        CP("gpsimd", PL[:], Pt[:, :, 127], [Pt.b], [PL.b])
        yield
        for f in range(4):
            ACT(KK[:, f, :], XS[:, 4 + f, :], AF.Copy, [XS.b, pfm.b], [KK.b], scale=pfm[:, 2, f:f + 1])
        ACT(sq_b[:], KK[:], AF.Square, [KK.b], [sq_b.b])
        bk = bank()
        MM(pf32(bk), blk_b, sq_b[:].rearrange("p a b -> p (a b)"), [cst_b.b, sq_b.b], [psb[bk]])
        TS("vector", rn[:].rearrange("p a b -> p (a b)"), pf32(bk), 1e-24, None, ALU.max, None, [psb[bk]], [rn.b])
        RSQ(rn[:], rn[:], rn.b)
        yield
        TT("vector", KK[:], KK[:], rn[:], ALU.mult, [KK.b, rn.b], [KK.b])
        for f in range(4):
            ACT(tmpk[:, f, :], alr[:, f, :], AF.Identity, [alr.b, pfm.b, omka.b, tmpk.b], [tmpk.b],
                scale=pfm[:, 3, f:f + 1], bias=omka[:, f:f + 1])
        TT("vector", kmod[:], XS[:, 4:8, :], tmpk[:], ALU.mult, [XS.b, tmpk.b], [kmod.b])
        TT("gpsimd", AR[:, :, 1, :], XS[:, 0:4, :], Pt[:], ALU.mult, [XS.b, Pt.b], [AR.b])
        STT("vector", AR[:, :, 0, :], KK[:], -1.0, Pm1[:], ALU.mult, ALU.mult, [KK.b, Pm1.b, AR.b], [AR.b])
        yield
        TT("vector", rn[:], KK[:], alr[:], ALU.mult, [KK.b, alr.b, rn.b], [rn.b])
        TT("vector", Bt[:], rn[:], iP[:], ALU.mult, [rn.b, iP.b], [Bt.b])
        TT("vector", Kt[:], kmod[:], iP[:], ALU.mult, [kmod.b, iP.b], [Kt.b])
        CP("gpsimd", vb[:], XS[:, 8:12, :], [XS.b], [vb.b])
        yield
        for f in range(4):
            STT("vector", rkb[:, f, :], XS[:, f, :], pfm[:, 4, f:f + 1], kmod[:, f, :], ALU.mult, ALU.mult,
                [XS.b, pfm.b, kmod.b], [rkb.b])
        bk = bank()
        MM(pf32(bk), blk_b, rkb[:].rearrange("p a b -> p (a b)"), [cst_b.b, rkb.b], [psb[bk]])
        TT("vector", BV[:].rearrange("p a b -> p (a b)"), pf32(bk), XS[:, 8:12, :].rearrange("p a b -> p (a b)"), ALU.mult,
           [psb[bk], XS.b], [BV.b])
        yield
        for src, dst in ((vb, Vtm), (Bt, BtT), (Kt, KtT)):
            bk = bank()
            for f in range(4):
                TR(pbf(bk)[:, f * 128:(f + 1) * 128], src[:, f, :], ident_b, [src.b, cst_b.b], [psb[bk]])
            CP("scalar", dst[:].rearrange("p a b -> p (a b)"), pbf(bk)[:, 0:512], [psb[bk]], [dst.b])
            yield
        for f in range(4):
            for hp in range(2):
                b1, b2, b3 = bank(), bank(), bank()
                pb = hp * 64
                h = 2 * f + hp
                arh = AR[pb:pb + 64, f, :, :].rearrange("p a b -> p (a b)")
                MM(pf32(b1)[:, 0:256], Bt[pb:pb + 64, f, :], arh, [Bt.b, AR.b], [psb[b1]])
                MM(pf32(b2)[:, 0:256], Kt[pb:pb + 64, f, :], arh, [Kt.b, AR.b], [psb[b2]])
                MM(pf32(b3)[:, 0:128], AR[pb:pb + 64, f, 0, :], Bt[pb:pb + 64, f, :], [Bt.b, AR.b], [psb[b3]])
                TT("vector", XA[:, h, :, :], pf32(b1)[:, 0:256].rearrange("p (a b) -> p a b", a=2), msk_2, ALU.mult,
                   [psb[b1], cst_f.b], [XA.b])
                TT("vector", KA2[:, h, :, :], pf32(b2)[:, 0:256].rearrange("p (a b) -> p a b", a=2), msk_2, ALU.mult,
                   [psb[b2], cst_f.b], [KA2.b])
                TT("vector", XT0[:, h, :], pf32(b3)[:, 0:128], msk_l, ALU.mult,
                   [psb[b3], cst_f.b], [XT0.b])
            yield

    def chain(ti):
        I = IF[ti % 2]
        AR, Vtm, BtT, KtT, XA, KA2, XT0, PL = (I[k] for k in ("AR", "Vtm", "BtT", "KtT", "XA", "KA2", "XT0", "PL"))
        TT("gpsimd", Mb[:], XA[:, :, 0, :], cst_f[:, 0:1, :].to_broadcast([128, 8, 128]), ALU.add, [XA.b, cst_f.b], [Mb.bq[0], Mb.bq[1]])

        def views(t, strided):
            return (lambda h: t[:, h, 0, :]) if strided else (lambda h: t[:, h, :])
        Xc, XTc, Xcb, XTcb = views(XA, True), views(XT0, False), [XA.b, XA.b], [XT0.b, XT0.b]
        for stp in range(7):
            do_prod = stp >= 1
            do_sq = stp <= 5
            bm, bx, bxt = [None, None], [None, None], [None, None]
            for q in range(2):
                hs = range(4 * q, 4 * q + 4)
                if do_prod:
                    bm[q] = bank()
                    for h in hs:
                        MM(pf32(bm[q])[:, (h % 4) * 128:(h % 4 + 1) * 128], XTc(h), Mb[:, h, :], [XTcb[q], Mb.bq[q]], [psb[bm[q]]])
                if do_sq:
                    bx[q], bxt[q] = bank(), bank()
                    for h in hs:
                        MM(pf32(bx[q])[:, (h % 4) * 128:(h % 4 + 1) * 128], XTc(h), Xc(h), [XTcb[q], Xcb[q]], [psb[bx[q]]])
                    for h in hs:
                        MM(pf32(bxt[q])[:, (h % 4) * 128:(h % 4 + 1) * 128], Xc(h), XTc(h), [XTcb[q], Xcb[q]], [psb[bxt[q]]])
            nx, nxt = Xp[stp % 2], XTp[stp % 2]
            for q in range(2):
                sl = slice(4 * q, 4 * q + 4)
                if do_prod:
                    TT("vector", Mb[:, sl, :], pf32(bm[q]).rearrange("p (a b) -> p a b", a=4), Mb[:, sl, :],
                       ALU.add, [psb[bm[q]], Mb.bq[q]], [Mb.bq[q]])
                if do_sq:
                    CP("scalar", nx[:, sl, :], pf32(bx[q]).rearrange("p (a b) -> p a b", a=4), [psb[bx[q]]], [nx.bq[q]])
                    CP("scalar", nxt[:, sl, :], pf32(bxt[q]).rearrange("p (a b) -> p a b", a=4), [psb[bxt[q]]], [nxt.bq[q]])
            if do_sq:
                Xc, XTc, Xcb, XTcb = views(nx, False), views(nxt, False), nx.bq, nxt.bq
            yield
        bW = bank()
        for h in range(8):
            f, pb = h // 2, (h % 2) * 64
            MM(pf32(bW)[:, h * 64:(h + 1) * 64], AR[pb:pb + 64, f, 0, :], ST_b[pb:pb + 64, f, :], [AR.b, ST_b.b], [psb[bW]],
               start=True, stop=False)
            MM(pf32(bW)[:, h * 64:(h + 1) * 64], KA2[:, h, 0, :], Vtm[:, f, (h % 2) * 64:(h % 2) * 64 + 64], [KA2.b, Vtm.b], [psb[bW]],
               start=False, stop=True)
        CP("scalar", W0T_b[:], pf32(bW), [psb[bW]], [W0T_b.b])
        yield
        bU = bank()
        for h in range(8):
            MM(pf32(bU)[:, h * 64:(h + 1) * 64], Mb[:, h, :], W0T_b[:, h * 64:(h + 1) * 64], [Mb.bq[h // 4], W0T_b.b], [psb[bU]])
        CP("vector", UT_b[:], pf32(bU), [psb[bU]], [UT_b.b])
        yield
        bY, bS = bank(), bank()
        for h in range(8):
            f, pb = h // 2, (h % 2) * 64
            hc = slice((h % 2) * 64, (h % 2) * 64 + 64)
            oy = ps[pb:pb + 64, bY, f * 128:(f + 1) * 128]
            MM(oy, ST_b[pb:pb + 64, f, :], AR[pb:pb + 64, f, 1, :], [ST_b.b, AR.b], [psb[bY]], start=True, stop=False)
            MM(oy, UT_b[:, h * 64:(h + 1) * 64], XA[:, h, 1, :], [UT_b.b, XA.b], [psb[bY]], start=False, stop=False)
            MM(oy, Vtm[:, f, hc], KA2[:, h, 1, :], [Vtm.b, KA2.b], [psb[bY]], start=False, stop=True)
        for h in range(8):
            f, pb = h // 2, (h % 2) * 64
            hc = slice((h % 2) * 64, (h % 2) * 64 + 64)
            os_ = ps[pb:pb + 64, bS, f * 64:(f + 1) * 64]
            MM(os_, BtT[:, f, hc], UT_b[:, h * 64:(h + 1) * 64], [BtT.b, UT_b.b], [psb[bS]], start=True, stop=False)
            MM(os_, KtT[:, f, hc], Vtm[:, f, hc], [KtT.b, Vtm.b], [psb[bS]], start=False, stop=True)
        CP("vector", y_f[:].rearrange("p a b -> p (a b)"), pf32(bY), [psb[bY]], [y_f.b])
        for f in range(4):
            ACT(ST_f[:, f, :], ST_f[:, f, :], AF.Copy, [ST_f.b, PL.b, ST_b.b], [ST_f.b], scale=PL[:, f:f + 1])
        for f in range(4):
            STT("vector", ST_f[:, f, :], ps[:, bS, f * 64:(f + 1) * 64], PL[:, f:f + 1], ST_f[:, f, :], ALU.mult, ALU.add,
                [psb[bS], PL.b, ST_f.b], [ST_f.b])
        CP("gpsimd", ST_b[:], ST_f[:], [ST_f.b], [ST_b.b])
        yield

    def gn(ti):
        I = IF[ti % 2]
        g_f, BV = I["g_f"], I["BV"]
        bk = bank()
        MM(pf32(bk), blk_f, y_f[:].rearrange("p a b -> p (a b)"), [cst_f.b, y_f.b], [psb[bk]])
        STT("vector", yd[:].rearrange("p a b -> p (a b)"), pf32(bk), -1.0 / 64, y_f[:].rearrange("p a b -> p (a b)"),
            ALU.mult, ALU.add, [psb[bk], y_f.b, yd.b], [yd.b])
        yield
        ACT(yd2[:], yd[:], AF.Square, [yd.b, yd2.b], [yd2.b])
        yield
        bk = bank()
        MM(pf32(bk), blk_f, yd2[:].rearrange("p a b -> p (a b)"), [cst_f.b, yd2.b], [psb[bk]])
        TS("vector", yd2[:].rearrange("p a b -> p (a b)"), pf32(bk), 1.0 / 64, 64e-5, ALU.mult, ALU.add,
           [psb[bk], yd2.b], [yd2.b])
        yield
        RSQ(yd2[:], yd2[:], yd2.b)
        yield
        TT("vector", yd[:], yd[:], yd2[:], ALU.mult, [yd.b, yd2.b], [yd.b])
        for f in range(4):
            ACT(yd[:, f, :], yd[:, f, :], AF.Identity, [yd.b, pfm.b], [yd.b], scale=pfm[:, 5, f:f + 1], bias=pfm[:, 6, f:f + 1])
        yield
        TT("vector", yd[:], yd[:], BV[:], ALU.add, [yd.b, BV.b], [yd.b])
        TT("vector", orw_b[:], yd[:], g_f[:], ALU.mult, [yd.b, g_f.b], [orw_b.b])
        DMA("sync", orw_d[ti], orw_b[:].rearrange("p a b -> p (a b)"), [orw_b.b], [orw_db])
        if dbg and stage == 1:
            DMA("sync", dbg_d[ti * 128:(ti + 1) * 128, 0:512], y_f[:].rearrange("p a b -> p (a b)"), [y_f.b], [dbgb])
        yield

    def gm(ti):
        zu, zv = ZU[ti % 2], ZV[ti % 2]
        dvf = dv[:].rearrange("p a b -> p (a b)")
        RSUM("vector", gst[:, 0, :], zv[:], [zv.b], [gst.b])
        TS("vector", gst[:, 1, :], gst[:, 0, :], -1.0 / 64, None, ALU.mult, None, [gst.b], [gst.b])
        TT("gpsimd", dv[:], zv[:], gst[:, 1, :].unsqueeze(2).to_broadcast([128, 8, 64]), ALU.add, [zv.b, gst.b], [dv.b])
        yield
        ACT(dv2[:], dv[:], AF.Square, [dv.b], [dv2.b])
        RSUM("vector", gst[:, 2, :], dv2[:], [dv2.b], [gst.b])
        TS("vector", gst[:, 3, :], gst[:, 2, :], 1.0 / 64, 1e-5, ALU.mult, ALU.add, [gst.b], [gst.b])
        RSQ(gst[:, 3, :], gst[:, 3, :], gst.b)
        yield
        TT("gpsimd", dv[:], dv[:], gst[:, 3, :].unsqueeze(2).to_broadcast([128, 8, 64]), ALU.mult, [dv.b, gst.b], [dv.b])
        TT("gpsimd", dvf, dvf, lng[:], ALU.mult, [dv.b, lng.b], [dv.b])
        TT("gpsimd", vn_b[:], dvf, lnb[:], ALU.add, [dv.b, lnb.b], [vn_b.b])
        yield
        bk = bank()
        for g in range(8):
            MM(pf32(bk)[:, g * 64:(g + 1) * 64], wsT_b[:, g, :], vn_b[:, g * 64:(g + 1) * 64], [wsT_b.b, vn_b.b], [psb[bk]])
        TT("vector", dv2[:], pf32(bk).rearrange("p (a b) -> p a b", a=8), bs_tm[:].unsqueeze(2).to_broadcast([128, 8, 64]), ALU.add,
           [psb[bk], bs_tm.b, dv2.b], [dv2.b])
        yield
        TT("gpsimd", ogm_b[:], dv2[:].rearrange("p a b -> p (a b)"), zu[:], ALU.mult, [dv2.b, zu.b, ogm_b.b], [ogm_b.b])
        transpose8(ogm_b, ogT_b, n=4)
        DMA("sync", ogT_d[ti], ogT_b[:].rearrange("p a b -> p (a b)"), [ogT_b.b], [ogT_db])
        if dbg and stage == 1:
            DMA("sync", dbg_d[ti * 128:(ti + 1) * 128, 512:1024], zu[:], [zu.b], [dbgb])
        yield

    def interleave(*gens):
        gens = [g for g in gens if g is not None]
        while gens:
            for g in list(gens):
                try:
                    next(g)
                except StopIteration:
                    gens.remove(g)

    interleave(front(0))
    interleave(prep(0), gm(0))
    if NT > 1:
        interleave(front(1))
    for ti in range(NT):
        interleave(chain(ti), prep(ti + 1) if ti + 1 < NT else None, gm(ti + 1) if ti + 1 < NT else None)
        interleave(gn(ti), front(ti + 2) if ti + 2 < NT else None)

    if stage == 1:
        P.emit(nc)
        return nc, st

    barrier()
    PH.reset()
    wts_all = PERS.alloc("wts_all", [128, NT, NE])
    d8_all = PERS.alloc("d8_all", [128, NT, 8], I32)
    w8_all = PERS.alloc("w8_all", [128, NT, 8])
    widx = PERS.alloc("widx", [128, NQ * 128], I32)
    winG = PH.alloc("winG", [128, 8, 2048], BF16)
    for kc in range(8):
        DMA("gpsimd", winG[:, kc, :], winv[:, kc, 2816:INC], [], [winG.b])
    wbr_rw = PH.alloc("wbr_rw", [128, 4, D], BF16)
    wbr_gm = PH.alloc("wbr_gm", [128, 4, D], BF16)
    wout_b = PH.alloc("wout_b", [128, 8, D], BF16)
    DMA("gpsimd", wbr_rw[:], wbr_rw_d.rearrange("(kc p) n -> p kc n", p=128), [], [wbr_rw.b])
    DMA("gpsimd", wbr_gm[:], wbr_gm_d.rearrange("(kc p) n -> p kc n", p=128), [], [wbr_gm.b])
    DMA("gpsimd", wout_b[:], wout_d.rearrange("(kc p) n -> p kc n", p=128), [], [wout_b.b])
    rw_b = PH.alloc("rw_b", [128, 8, NE], BF16)
    shgu = PH.alloc("shgu", [128, 8, 512], BF16)
    shd_b = PH.alloc("shd_b", [128, 2, D], BF16)
    DMA("gpsimd", rw_b[:], rw_d.rearrange("(kc p) n -> p kc n", p=128), [], [rw_b.b])
    DMA("gpsimd", shgu[:, :, 0:256], sg_d.rearrange("(kc p) n -> p kc n", p=128), [], [shgu.b])
    DMA("gpsimd", shgu[:, :, 256:512], su_d.rearrange("(kc p) n -> p kc n", p=128), [], [shgu.b])
    DMA("gpsimd", shd_b[:], sd_d.rearrange("(kc p) n -> p kc n", p=128), [], [shd_b.b])
    rb_rep = PH.alloc("rb_rep", [128, NE])
    DMA("sync", rb_rep[:], rb_d.partition_broadcast(128), [], [rb_rep.b])
    XT2 = [PH.alloc("xt2_%d" % i, [128, D]) for i in range(2)]
    hT2 = [PH.alloc("hT2_%d" % i, [128, 8, 128], BF16) for i in range(2)]
    orw2 = [PH.alloc("orw2_%d" % i, [128, 4, 128], BF16) for i in range(2)]
    ogT2 = [PH.alloc("ogT2_%d" % i, [128, 4, 128], BF16) for i in range(2)]
    Gs = PH.alloc("Gs", [128, 16, 128])
    t1 = PH.alloc("t1", [128, 8, 128])
    t2 = PH.alloc("t2", [128, 8, 128])
    mT_b = PH.alloc("mT_b", [128, 8, 128], BF16)
    tmpo = PH.alloc("tmpo", [128, D])
    x1t = PH.alloc("x1t", [128, D])
    X1 = [x1t, Tl(gs1[:], "x1t_b")]
    tmpoR = Tl(modr[:, 0, :], "tmpoR")
    sqf2 = Tl(modr[:, 1, :], "sqf2")
    acc_t = Tl(modr[:, 4, :], "acc_t")
    ssn2 = PH.alloc("ssn2", [128, 4])
    h2_tm = PH.alloc("h2_tm", [128, D], BF16)
    h2T = PH.alloc("h2T", [128, 8, 128], BF16)
    sc = PH.alloc("sc", [128, NE])
    ssel = PH.alloc("ssel", [128, NE])
    sgl = PH.alloc("sgl", [128, 256])
    top8 = PH.alloc("top8", [128, 8])
    rsm = PH.alloc("rsm", [128, 2])
    actT = PH.alloc("actT", [128, 2, 128], BF16)

    def load_ii(tj):
        DMA("sync", hT2[tj % 2][:].rearrange("p a b -> p (a b)"), hT_d[tj], [hT_db], [hT2[tj % 2].b])
        DMA("sync", orw2[tj % 2][:].rearrange("p a b -> p (a b)"), orw_d[tj], [orw_db], [orw2[tj % 2].b])
        DMA("sync", ogT2[tj % 2][:].rearrange("p a b -> p (a b)"), ogT_d[tj], [ogT_db], [ogT2[tj % 2].b])
        DMA("sync", XT2[tj % 2][:], x_d[tj * 128:(tj + 1) * 128, :], [], [XT2[tj % 2].b])

    def Gpart(ti):
        xt, hTt, orw_t, ogT_t, x1 = XT2[ti % 2], hT2[ti % 2], orw2[ti % 2], ogT2[ti % 2], X1[ti % 2]
        for q in range(4):
            bk = bank()
            for f in range(4):
                ft = q * 4 + f
                for kc in range(8):
                    MM(pf32(bk)[:, f * 128:(f + 1) * 128], winG[:, kc, ft * 128:(ft + 1) * 128], hTt[:, kc, :], [winG.b, hTt.b],
                       [psb[bk]], start=(kc == 0), stop=(kc == 7))
            ACT(Gs[:, q * 4:(q + 1) * 4, :].rearrange("p a b -> p (a b)"), pf32(bk), AF.Sigmoid, [psb[bk]], [Gs.b])
            yield
        for wbr, src, dst, go in ((wbr_rw, orw_t, t1, 0), (wbr_gm, ogT_t, t2, 8)):
            for q in range(2):
                bk = bank()
                for f in range(4):
                    ft = q * 4 + f
                    for kc in range(4):
                        MM(pf32(bk)[:, f * 128:(f + 1) * 128], wbr[:, kc, ft * 128:(ft + 1) * 128], src[:, kc, :], [wbr.b, src.b],
                           [psb[bk]], start=(kc == 0), stop=(kc == 3))
                TT("vector", dst[:, q * 4:(q + 1) * 4, :].rearrange("p a b -> p (a b)"), pf32(bk),
                   Gs[:, go + q * 4:go + (q + 1) * 4, :].rearrange("p a b -> p (a b)"), ALU.mult, [psb[bk], Gs.b], [dst.b])
                yield
        TT("vector", mT_b[:, 0:4, :], t1[:, 0:4, :], t2[:, 0:4, :], ALU.add, [t1.b, t2.b], [mT_b.b])
        TT("vector", mT_b[:, 4:8, :], t1[:, 4:8, :], t2[:, 4:8, :], ALU.add, [t1.b, t2.b], [mT_b.b])
        for n in range(2):
            bk = bank()
            for kc in range(8):
                MM(pf32(bk), mT_b[:, kc, :], wout_b[:, kc, n * 512:(n + 1) * 512], [mT_b.b, wout_b.b], [psb[bk]],
                   start=(kc == 0), stop=(kc == 7))
            TT("vector", tmpo[:, n * 512:(n + 1) * 512], pf32(bk), gt1[:, n * 512:(n + 1) * 512], ALU.mult, [psb[bk], modr.b], [tmpo.b])
            if n == 0:
                yield
        TT("vector", x1[:], tmpo[:], xt[:], ALU.add, [tmpo.b, xt.b], [x1.b])
        if dbg and stage == 2:
            DMA("sync", dbg_d[ti * 128:(ti + 1) * 128, :], x1[:], [x1.b], [dbgb])
        yield

    def Rpart(ti):
        x1 = X1[ti % 2]
        rmsnorm_rstd(x1, sqf2, ssn2)
        STT("vector", sqf2[:], x1[:], ssn2[:, 2:3], gs2[:], ALU.mult, ALU.mult, [x1.b, ssn2.b, gs2.b, sqf2.b], [sqf2.b])
        TT("vector", h2_tm[:], sqf2[:], sh2, ALU.add, [sqf2.b, modr.b], [h2_tm.b])
        DMA("sync", h2_d[ti * 128:(ti + 1) * 128, :], h2_tm[:], [h2_tm.b], [h2_db])
        yield
        transpose8(h2_tm, h2T)
        yield
        bk = bank()
        for kc in range(8):
            MM(pf32(bk)[:, 0:NE], h2T[:, kc, :], rw_b[:, kc, :], [h2T.b, rw_b.b], [psb[bk]], start=(kc == 0), stop=(kc == 7))
        ACT(sc[:], pf32(bk)[:, 0:NE], AF.Sigmoid, [psb[bk]], [sc.b])
        yield
        TT("gpsimd", ssel[:], sc[:], rb_rep[:], ALU.add, [sc.b, rb_rep.b], [ssel.b])
        MAX8(top8[:], ssel[:], [ssel.b], [top8.b])
        TS("vector", ssel[:], ssel[:], top8[:, 7:8], None, ALU.is_ge, None, [ssel.b, top8.b], [ssel.b])
        TT("gpsimd", sc[:], sc[:], ssel[:], ALU.mult, [sc.b, ssel.b], [sc.b])
        RSUM("vector", rsm[:, 0:1], sc[:], [sc.b], [rsm.b])
        P.add("vector", lambda e: e.reciprocal(out=rsm[:, 1:2], in_=rsm[:, 0:1]), reads=[rsm.b], writes=[rsm.b])
        TS("vector", wts_all[:, ti, :], sc[:], rsm[:, 1:2], 2.5, ALU.mult, ALU.mult, [sc.b, rsm.b], [wts_all.b])
        yield
        bk = bank()
        for j in range(4):
            for kc in range(8):
                MM(pf32(bk)[:, j * 128:(j + 1) * 128], shgu[:, kc, j * 128:(j + 1) * 128], h2T[:, kc, :], [shgu.b, h2T.b], [psb[bk]],
                   start=(kc == 0), stop=(kc == 7))
        ACT(sgl[:], pf32(bk)[:, 0:256], AF.Silu, [psb[bk]], [sgl.b])
        TT("vector", actT[:].rearrange("p a b -> p (a b)"), pf32(bk)[:, 256:512], sgl[:], ALU.mult, [psb[bk], sgl.b], [actT.b])
        yield
        for n in range(2):
            bk = bank()
            for fc in range(2):
                MM(pf32(bk), actT[:, fc, :], shd_b[:, fc, n * 512:(n + 1) * 512], [actT.b, shd_b.b], [psb[bk]],
                   start=(fc == 0), stop=(fc == 1))
            TT("vector", tmpoR[:, n * 512:(n + 1) * 512], pf32(bk), gt2[:, n * 512:(n + 1) * 512], ALU.mult,
               [psb[bk], modr.b, tmpoR.b], [tmpoR.b])
            if n == 0:
                yield
        TT("gpsimd", acc_t[:], tmpoR[:], x1[:], ALU.add, [tmpoR.b, x1.b], [acc_t.b])
        DMA("sync", acc_d[ti * 128:(ti + 1) * 128, :], acc_t[:], [acc_t.b], [acc_db])
        yield

    def interleave2(*gens):
        gens = [g for g in gens if g is not None]
        while gens:
            for g in list(gens):
                try:
                    next(g)
                except StopIteration:
                    gens.remove(g)

    load_ii(0)
    if NT > 1:
        load_ii(1)
    interleave2(Gpart(0))
    for ti in range(NT):
        if ti + 2 < NT:
            load_ii(ti + 2)
        interleave2(Rpart(ti), Gpart(ti + 1) if ti + 1 < NT else None)

    if stage == 2:
        P.emit(nc)
        return nc, st

    barrier()
    PH.reset()
    iota1 = PH.alloc("iota1", [128, NE])
    DMA("sync", iota1[:], iota_d.partition_broadcast(128), [], [iota1.b])
    mk_all = PH.alloc("mk_all", [128, NT, NE], BF16)
    cumx = PH.alloc("cumx", [128, NT + 1, NE], BF16)
    TS("gpsimd", mk_all[:], wts_all[:], 0.0, None, ALU.is_gt, None, [wts_all.b], [mk_all.b])
    MSET("gpsimd", cumx[:, 0, :], 0.0, [cumx.b])
    for ti in range(NT):
        TT("gpsimd", cumx[:, ti + 1, :], cumx[:, ti, :], mk_all[:, ti, :], ALU.add, [cumx.b, mk_all.b], [cumx.b])
    cnt = PH.alloc("cnt", [128, NE])
    pad = PH.alloc("pad", [128, NE])
    pends = PH.alloc("pends", [128, NE])
    pstart = PH.alloc("pstart", [128, NE])
    bk = bank()
    MM(pf32(bk)[:, 0:NE], ones_b[:], cumx[:, NT, :], [ones_b.b, cumx.b], [psb[bk]])
    CP("vector", cnt[:], pf32(bk)[:, 0:NE], [psb[bk]], [cnt.b])
    MSET("vector", pad[:], 0.0, [pad.b])
    for m in range(NT):
        STT("vector", pad[:], cnt[:], float(128 * m), pad[:], ALU.is_gt, ALU.add, [cnt.b, pad.b], [pad.b])
    TS("vector", pad[:], pad[:], 128.0, None, ALU.mult, None, [pad.b], [pad.b])
    P.add("vector", lambda e: e.tensor_tensor_scan(out=pends[:], data0=ones_f[:], data1=pad[:], initial=0.0, op0=ALU.mult, op1=ALU.add),
          reads=[pad.b, ones_f.b], writes=[pends.b])
    TT("vector", pstart[:], pends[:], pad[:], ALU.subtract, [pends.b, pad.b], [pstart.b])
    be = PH.alloc("be", [128, NQ])
    thr = PH.alloc("thr", [128, NQ])
    cmpj = PH.alloc("cmpj", [128, NE])
    Dg = PH.alloc("Dg", [128, NQ, 128])
    for q in range(NQ):
        TS("vector", thr[:, q:q + 1], misc[:, 0:1], 128.0, float(q * 128 * 128), ALU.mult, ALU.add, [misc.b], [thr.b])
        TS("vector", cmpj[:], pends[:], thr[:, q:q + 1], None, ALU.is_le, None, [pends.b, thr.b, cmpj.b], [cmpj.b])
        RSUM("vector", be[:, q:q + 1], cmpj[:], [cmpj.b], [be.b])
        TS("gpsimd", Dg[:, q, :], cst_f[:, 0, :], be[:, q:q + 1], None, ALU.mult, None, [cst_f.b, be.b], [Dg.b])
    NBP = NQ * 128
    ber = PH.alloc("ber", [128, NBP + 1])
    sameb = PH.alloc("sameb", [128, NBP])
    widf = PH.alloc("widf", [128, NBP])
    MSET("vector", ber[:, 0:1], -1.0, [ber.b])
    for q in range(NQ):
        bk = bank()
        MM(pf32(bk)[:, 0:128], ones_f[:, 0:128], Dg[:, q, :], [ones_f.b, Dg.b], [psb[bk]])
        CP("vector", ber[:, 1 + q * 128:1 + (q + 1) * 128], pf32(bk)[:, 0:128], [psb[bk], ber.b], [ber.b])
    TT("vector", sameb[:], ber[:, 1:NBP + 1], ber[:, 0:NBP], ALU.is_equal, [ber.b], [sameb.b])
    TS("vector", widf[:], ber[:, 1:NBP + 1], 128.0, misc[:, 0:1], ALU.mult, ALU.add, [ber.b, misc.b], [widf.b])
    STT("vector", widx[:], sameb[:], 1048576.0, widf[:], ALU.mult, ALU.add, [sameb.b, widf.b], [widx.b])
    dst1 = PH.alloc("dst1", [128, NE])
    km = PH.alloc("km", [128, NE])
    d8f = PH.alloc("d8f", [128, 8])
    e8 = PH.alloc("e8", [128, 8])
    eq8 = PH.alloc("eq8", [128, 8, NE])
    h2l = [PH.alloc("h2l%d" % i, [128, D], BF16) for i in range(2)]
    d8_all.bt = [Buf("d8t") for _ in range(NT)]
    w8_all.bt = [Buf("w8t") for _ in range(NT)]
    DMA("sync", h2l[0][:], h2_d[0:128, :], [h2_db], [h2l[0].b])
    for ti in range(NT):
        if ti + 1 < NT:
            DMA("sync", h2l[(ti + 1) % 2][:], h2_d[(ti + 1) * 128:(ti + 2) * 128, :], [h2_db], [h2l[(ti + 1) % 2].b])
        bk = bank()
        MM(pf32(bk)[:, 0:NE], cst_b[:, 1, :], mk_all[:, ti, :], [cst_b.b, mk_all.b], [psb[bk]], start=True, stop=False)
        MM(pf32(bk)[:, 0:NE], ones_b[:], cumx[:, ti, :], [ones_b.b, cumx.b], [psb[bk]], start=False, stop=True)
        STT("vector", dst1[:], pf32(bk)[:, 0:NE], 1.0, pstart[:], ALU.add, ALU.add, [psb[bk], pstart.b, dst1.b], [dst1.b])
        TT("vector", dst1[:], dst1[:], mk_all[:, ti, :], ALU.mult, [dst1.b, mk_all.b], [dst1.b])
        MAX8(d8f[:], dst1[:], [dst1.b], [d8f.b])
        TS("vector", d8_all[:, ti, :], d8f[:], -1.0, None, ALU.add, None, [d8f.b], [d8_all.bt[ti]])
        TT("vector", km[:], iota1[:], mk_all[:, ti, :], ALU.mult, [iota1.b, mk_all.b, km.b], [km.b])
        MAX8(e8[:], km[:], [km.b], [e8.b])
        for k in range(8):
            STT("vector", eq8[:, k, :], km[:], e8[:, k:k + 1], wts_all[:, ti, :], ALU.is_equal, ALU.mult,
                [km.b, e8.b, wts_all.b, eq8.b], [eq8.b])
        RSUM("vector", w8_all[:, ti, :], eq8[:], [eq8.b], [w8_all.bt[ti]])
        h2t = h2l[ti % 2]
        for k in range(8):
            P.add("gpsimd", (lambda ix, src: lambda e: e.indirect_dma_start(
                out=xs_d, out_offset=bass.IndirectOffsetOnAxis(ap=ix, axis=0), in_=src, in_offset=None))(d8_all[:, ti, k:k + 1], h2t[:]),
                reads=[h2t.b, d8_all.bt[ti]], writes=[Buf("xs_part")], dma=True)

    barrier()
    PH.reset()
    wgb = [PH.alloc("wgb%d" % i, [128, 8, 256], BF16) for i in range(2)]
    wub = [PH.alloc("wub%d" % i, [128, 8, 256], BF16) for i in range(2)]
    wdb = [PH.alloc("wdb%d" % i, [128, 2, D], BF16) for i in range(2)]
    wgf = [PH.alloc("wgf", [128, 2048])] * 2
    wuf = [PH.alloc("wuf", [128, 2048])] * 2
    wdf = [PH.alloc("wdf", [128, 2048])] * 2
    xbl = [PH.alloc("xbl%d" % i, [128, D], BF16) for i in range(2)]
    xbT = [PH.alloc("xbT%d" % i, [128, 8, 128], BF16) for i in range(2)]
    sgl2 = PH.alloc("sgl2", [128, 256])
    actE = PH.alloc("actE", [128, 2, 128], BF16)
    obt = [PH.alloc("obt%d" % i, [128, D]) for i in range(2)]
    _breg = {}

    def bound_reg(e):
        if "r" not in _breg:
            _breg["r"] = e.to_reg(NE * 128 - 1)
        return _breg["r"]

    def e_load(blk):
        DMA("sync", xbl[blk % 2][:], xs_d[blk * 128:(blk + 1) * 128, :], [], [xbl[blk % 2].b])

    def e_gather(blk):
        for wt, src in ((wgf[blk % 2], mg_d), (wuf[blk % 2], mup_d), (wdf[blk % 2], md_d)):
            P.add("gpsimd", (lambda o_, s_, ix: lambda e: e.indirect_dma_start(
                out=o_, out_offset=None, in_=s_, in_offset=bass.IndirectOffsetOnAxis(ap=ix, axis=0),
                bounds_check=bound_reg(e), oob_is_err=False))(wt[:], src, widx[:, blk:blk + 1]),
                reads=[widx.b], writes=[wt.b], dma=True)

    def e_cast(blk):
        wg, wu, wd = wgb[blk % 2], wub[blk % 2], wdb[blk % 2]
        wgs, wus, wds = wgf[blk % 2], wuf[blk % 2], wdf[blk % 2]
        CP("vector", wg[:].rearrange("p a b -> p (a b)"), wgs[:], [wgs.b], [wg.b])
        CP("vector", wu[:].rearrange("p a b -> p (a b)"), wus[:], [wus.b], [wu.b])
        CP("scalar", wd[:, 0, :], wds[:, 0:D], [wds.b], [wd.b])
        CP("scalar", wd[:, 1, :], wds[:, D:2 * D], [wds.b], [wd.b])

    def e_tr_pe(blk):
        bk = bank()
        xb = xbl[blk % 2]
        for kc in range(8):
            TR(pbf(bk)[:, kc * 128:(kc + 1) * 128], xb[:, kc * 128:(kc + 1) * 128], ident_b, [xb.b, cst_b.b], [psb[bk]])
        return bk

    def e_tr_cp(blk, bk):
        CP("scalar", xbT[blk % 2][:].rearrange("p a b -> p (a b)"), pbf(bk)[:, 0:1024], [psb[bk]], [xbT[blk % 2].b])

    e_load(0)
    e_load(1)
    e_gather(0)
    e_cast(0)
    e_gather(1)
    e_tr_cp(0, e_tr_pe(0))
    for blk in range(NBLK):
        wg, wu, wd, ob, xt_ = wgb[blk % 2], wub[blk % 2], wdb[blk % 2], obt[blk % 2], xbT[blk % 2]
        bk = bank()
        for j in range(4):
            wsrc = wg if j < 2 else wu
            for kc in range(8):
                MM(pf32(bk)[:, j * 128:(j + 1) * 128], wsrc[:, kc, (j % 2) * 128:(j % 2 + 1) * 128], xt_[:, kc, :], [wsrc.b, xt_.b],
                   [psb[bk]], start=(kc == 0), stop=(kc == 7))
        bkT = e_tr_pe(blk + 1) if blk + 1 < NBLK else None
        if blk + 1 < NBLK:
            e_cast(blk + 1)
        if blk + 2 < NBLK:
            e_load(blk + 2)
            e_gather(blk + 2)
        ACT(sgl2[:], pf32(bk)[:, 0:256], AF.Silu, [psb[bk]], [sgl2.b])
        TT("vector", actE[:].rearrange("p a b -> p (a b)"), pf32(bk)[:, 256:512], sgl2[:], ALU.mult, [psb[bk], sgl2.b], [actE.b])
        if blk + 1 < NBLK:
            e_tr_cp(blk + 1, bkT)
        for n in range(2):
            bk = bank()
            for fc in range(2):
                MM(pf32(bk), actE[:, fc, :], wd[:, fc, n * 512:(n + 1) * 512], [actE.b, wd.b], [psb[bk]], start=(fc == 0), stop=(fc == 1))
            CP("scalar" if n else "vector", ob[:, n * 512:(n + 1) * 512], pf32(bk), [psb[bk]], [ob.b])
        DMA("sync", ob_d[blk * 128:(blk + 1) * 128, :], ob[:], [ob.b], [Buf("ob_part")])

    barrier()
    NGK = 8
    gk = [PH.alloc("gk%d" % i, [128, D]) for i in range(NGK)]
    accl = [PH.alloc("accl%d" % i, [128, D]) for i in range(2)]
    ssums = [PH.alloc("ssum%d" % i, [128, D]) for i in range(2)]
    sqf3 = PH.alloc("sqf3", [128, D])
    ssn3 = PH.alloc("ssn3", [128, 4])
    outt = [PH.alloc("outt%d" % i, [128, D]) for i in range(2)]
    fing = PH.alloc("fing", [128, D])
    DMA("sync", fing[:], fing_d.partition_broadcast(128), [], [fing.b])
    gi = 0
    DMA("sync", accl[0][:], acc_d[0:128, :], [acc_db], [accl[0].b])
    for ti in range(NT):
        ac = accl[ti % 2]
        ssum = ssums[ti % 2]
        if ti + 1 < NT:
            DMA("sync", accl[(ti + 1) % 2][:], acc_d[(ti + 1) * 128:(ti + 2) * 128, :], [acc_db], [accl[(ti + 1) % 2].b])
        for k in range(8):
            g = gk[gi % NGK]
            gi += 1
            P.add("gpsimd", (lambda o_, ix: lambda e: e.indirect_dma_start(
                out=o_, out_offset=None, in_=ob_d, in_offset=bass.IndirectOffsetOnAxis(ap=ix, axis=0)))(g[:], d8_all[:, ti, k:k + 1]),
                reads=[d8_all.bt[ti]], writes=[g.b], dma=True)
            if k == 0:
                ACT(ssum[:], g[:], AF.Copy, [g.b, w8_all.bt[ti], ssum.b], [ssum.b], scale=w8_all[:, ti, 0:1])
            elif k % 2 == 0:
                ACT(g[:], g[:], AF.Copy, [g.b, w8_all.bt[ti]], [g.b], scale=w8_all[:, ti, k:k + 1])
                TT("vector", ssum[:], ssum[:], g[:], ALU.add, [g.b, ssum.b], [ssum.b])
            else:
                STT("vector", ssum[:], g[:], w8_all[:, ti, k:k + 1], ssum[:], ALU.mult, ALU.add, [g.b, w8_all.bt[ti], ssum.b], [ssum.b])
        TT("vector", ssum[:], ssum[:], gt2, ALU.mult, [ssum.b, modr.b], [ssum.b])
        TT("vector", ssum[:], ssum[:], ac[:], ALU.add, [ssum.b, ac.b], [ssum.b])
        rmsnorm_rstd(ssum, sqf3, ssn3)
        ot = outt[ti % 2]
        STT("vector", ot[:], ssum[:], ssn3[:, 2:3], fing[:], ALU.mult, ALU.mult, [ssum.b, ssn3.b, fing.b], [ot.b])
        DMA("sync", out_d[ti * 128:(ti + 1) * 128, :], ot[:], [ot.b], [Buf("out_part")])
    P.emit(nc)
    return nc, st


def _consts():
    i = np.arange(128)
    ident = np.eye(128, dtype=np.float32)
    strict = (i[:, None] < i[None, :]).astype(np.float32)
    incl = (i[:, None] <= i[None, :]).astype(np.float32)
    lower = (i[:, None] > i[None, :]).astype(np.float32)
    blk = ((i[:, None] // 64) == (i[None, :] // 64)).astype(np.float32)
    cst = np.stack([ident, strict, incl, lower, blk], axis=1)
    misc = np.zeros((128, 4), np.float32)
    misc[:, 0] = i
    return np.ascontiguousarray(cst), misc


def _shared_maps(inp):
    f = lambda a: np.ascontiguousarray(np.asarray(a, dtype=np.float32))
    cst, misc = _consts()
    fm4 = lambda v: np.asarray(v, np.float32).reshape(4, 128).T
    pf = np.stack([fm4(inp["rw_w0"][0]), fm4(inp["rw_a0"][0]), fm4(inp["rw_k_k"][0]), fm4(inp["rw_k_a"][0]),
                   fm4(inp["rw_r_k"][0].reshape(-1)), fm4(inp["rw_lnx_g"][0]), fm4(inp["rw_lnx_b"][0])], axis=1)
    mg = np.asarray(inp["moe_w_gate"][0], np.float32).reshape(NE, 8, 128, 256).transpose(0, 2, 1, 3).reshape(NE * 128, 2048)
    mu_ = np.asarray(inp["moe_w_up"][0], np.float32).reshape(NE, 8, 128, 256).transpose(0, 2, 1, 3).reshape(NE * 128, 2048)
    md = np.asarray(inp["moe_w_down"][0], np.float32).reshape(NE, 2, 128, D).transpose(0, 2, 1, 3).reshape(NE * 128, 2048)
    m = {
        "w_ada": f(inp["w_ada"][0]), "b_ada": f(inp["b_ada"][0]), "norm1_g": f(inp["norm1_g"][0]),
        "norm2_g": f(inp["norm2_g"][0]), "final_g": f(inp["final_g"]), "w_in": f(inp["w_in"][0]),
        "mu_fm": f(np.asarray(inp["rw_mu"][0], np.float32).reshape(14, 128).T), "pf": f(pf),
        "rw_w2": f(inp["rw_w2"][0]), "rw_a2": f(inp["rw_a2"][0]), "rw_g2": f(inp["rw_g2"][0]),
        "gm_ln_g": f(np.asarray(inp["gm_ln_g"][0]).reshape(-1)), "gm_ln_b": f(np.asarray(inp["gm_ln_b"][0]).reshape(-1)),
        "wsT": f(np.asarray(inp["gm_w_s"][0], np.float32).transpose(2, 0, 1)), "bs_tm": f(np.asarray(inp["gm_b_s"][0]).T),
        "w_br_rwkv": f(inp["w_br_rwkv"][0]), "w_br_gmlp": f(inp["w_br_gmlp"][0]), "w_out": f(inp["w_out"][0]),
        "router_w": f(inp["router_w"][0]), "router_b": f(inp["router_b"][0]),
        "moe_wg": np.ascontiguousarray(mg), "moe_wu": np.ascontiguousarray(mu_), "moe_wd": np.ascontiguousarray(md),
        "sh_w_gate": f(inp["sh_w_gate"][0]), "sh_w_up": f(inp["sh_w_up"][0]), "sh_w_down": f(inp["sh_w_down"][0]),
        "cst": cst, "misc": misc, "iota1": np.arange(1, NE + 1, dtype=np.float32),
    }
    return m


def _core_map(shared, inp, b, S):
    m = dict(shared)
    m["x"] = np.ascontiguousarray(np.asarray(inp["x"][b, :S], np.float32))
    m["c_pk"] = np.ascontiguousarray(np.asarray(inp["c"][b], np.float32).reshape(8, 128).T)
    return m


def kernel(**inputs):
    B = inputs["x"].shape[0]
    S = inputs["x"].shape[1]
    nc, st = build(S)
    shared = _shared_maps(inputs)
    in_maps = [_core_map(shared, inputs, b, S) for b in range(B)]
    res = run_bass_kernel_spmd(nc, in_maps, core_ids=list(range(B)))
    st.close()
    return np.stack([r["out"] for r in res.results], axis=0).astype(np.float32)
```

```python
import contextlib
import numpy as np
import concourse.bass as bass
import concourse.mybir as mybir
from concourse.bass_utils import run_bass_kernel_spmd

F32 = mybir.dt.float32
BF16 = mybir.dt.bfloat16
I32 = mybir.dt.int32
AF = mybir.ActivationFunctionType
ALU = mybir.AluOpType
AX = mybir.AxisListType

D = 1024
SEQ = 4096
NE = 256
INC = 4864
RWC = 1792


class Buf:
    __slots__ = ("name", "lw", "rd")

    def __init__(self, name=""):
        self.name = name
        self.lw = None
        self.rd = []


class Op:
    __slots__ = ("eng", "fn", "deps", "needed", "dma", "tok", "sem", "line")

    def __init__(self, eng, fn, dma):
        self.eng = eng
        self.fn = fn
        self.deps = []
        self.needed = False
        self.dma = dma
        self.tok = None
        self.sem = None


class Prog:
    ENGS = ("sync", "scalar", "vector", "gpsimd", "tensor")

    def __init__(self, n_dma_sems=32):
        self.ops = []
        self.n_dma = n_dma_sems
        self.dma_rr = 0
        self.dma_last = [None] * n_dma_sems
        self.dma_cnt = [0] * n_dma_sems
        self.last_eng = {}
        self.bar = []
        self.gen = 0
        self.eng_gen = {}

    def barrier(self):
        lasts = [self.last_eng[e] for e in self.ENGS if e in self.last_eng]
        lasts += [d for d in self.dma_last if d is not None]
        self.bar = lasts
        self.gen += 1

    def add(self, eng, fn, reads=(), writes=(), dma=False):
        op = Op(eng, fn, dma)
        import sys as _s
        op.line = _s._getframe(2).f_lineno
        deps = []
        for b in reads:
            if b.lw is not None:
                deps.append(b.lw)
        for b in writes:
            if b.lw is not None:
                deps.append(b.lw)
            deps.extend(b.rd)
        if dma:
            j = self.dma_rr
            self.dma_rr = (self.dma_rr + 1) % self.n_dma
            if self.dma_last[j] is not None:
                deps.append(self.dma_last[j])
            self.dma_last[j] = op
            self.dma_cnt[j] += 1
            op.sem = ("dma", j)
            op.tok = 16 * self.dma_cnt[j]
            op.needed = True
        else:
            op.sem = ("eng", eng)
        if self.eng_gen.get(eng, 0) < self.gen:
            deps.extend(self.bar)
            self.eng_gen[eng] = self.gen
        if not dma:
            self.last_eng[eng] = op
        seen = set()
        for d in deps:
            if id(d) in seen or d is op:
                continue
            seen.add(id(d))
            if (not d.dma) and (not dma) and d.eng == eng and eng == "tensor":
                continue
            op.deps.append(d)
            d.needed = True
        for b in reads:
            if dma:
                b.rd.append(op)
            else:
                b.rd = [r for r in b.rd if r.dma or r.eng != eng]
                b.rd.append(op)
        for b in writes:
            b.lw = op
            b.rd = []
        self.ops.append(op)
        return op

    def emit(self, nc):
        import os
        if os.environ.get("TRUNC"):
            self.ops = self.ops[:int(os.environ["TRUNC"])]
            self.dma_cnt = [0] * self.n_dma
            for op in self.ops:
                if op.dma:
                    self.dma_cnt[op.sem[1]] += 1
        print("n_ops", len(self.ops), flush=True)
        cnt = {e: 0 for e in self.ENGS}
        for op in self.ops:
            if not op.dma and op.needed:
                cnt[op.eng] += 1
                op.tok = cnt[op.eng]
        with contextlib.ExitStack() as st:
            esem = {e: st.enter_context(nc.semaphore("p_" + e)) for e in self.ENGS}
            dsem = [st.enter_context(nc.semaphore("d_%d" % j)) for j in range(self.n_dma)]
            block = st.enter_context(nc.Block())

            def semof(op):
                return dsem[op.sem[1]] if op.sem[0] == "dma" else esem[op.sem[1]]

            def run(engname):
                def body(eng):
                    waited = {}
                    for op in self.ops:
                        if op.eng != engname:
                            continue
                        for d in op.deps:
                            if waited.get(d.sem, 0) >= d.tok:
                                continue
                            waited[d.sem] = d.tok
                            eng.wait_ge(semof(d), d.tok)
                        inst = op.fn(eng)
                        if op.dma:
                            inst.then_inc(semof(op), 16)
                        elif op.needed:
                            inst.then_inc(semof(op), 1)
                    if engname == "sync":
                        for j in range(self.n_dma):
                            if self.dma_cnt[j]:
                                eng.wait_ge(dsem[j], 16 * self.dma_cnt[j])
                return body

            block.sync(run("sync"))
            block.scalar(run("scalar"))
            block.vector(run("vector"))
            block.gpsimd(run("gpsimd"))
            block.tensor(run("tensor"))


class Tl:
    def __init__(self, t, name):
        self.t = t
        self.b = Buf(name)

    def __getitem__(self, k):
        return self.t[k]


class Arena:
    def __init__(self, nc, st, name, nbytes):
        self.t = st.enter_context(nc.sbuf_tensor(name, [128, nbytes // 2], BF16))
        self.cap = nbytes // 2
        self.off = 0
        self.pers = 0
        self.name = name

    def reset(self):
        self.off = self.pers

    def alloc(self, name, shape, dt=F32, pers=False):
        esz = 2 if dt == BF16 else 4
        n = 1
        for s in shape[1:]:
            n *= s
        ne = n * esz // 2
        ne += ne % 2
        if pers:
            assert self.off == self.pers, "persistent alloc only right after reset"
        assert self.off + ne <= self.cap, (self.name, name, self.off, ne, self.cap)
        ap = self.t[:, self.off:self.off + n * esz // 2]
        self.off += ne
        if pers:
            self.pers = self.off
        if esz == 4:
            ap = ap.bitcast(dt)
        if len(shape) == 3:
            ap = ap.rearrange("p (a b) -> p a b", a=shape[1])
        elif len(shape) == 4:
            ap = ap.rearrange("p (a b c) -> p a b c", a=shape[1], b=shape[2])
        return Tl(ap, name)


def build(S=SEQ, stage=9, dbg=False):
    NT = S // 128
    NBLK = NT * 8 + NE
    NQ = (NBLK + 127) // 128
    nc = bass.Bass("TRN2", target_bir_lowering=False)
    P = Prog()
    st = contextlib.ExitStack()

    def din(name, shape, dt=F32):
        return nc.dram_tensor(name, shape, dt, kind="ExternalInput").ap()

    x_d = din("x", [S, D])
    c_d = din("c_pk", [128, 8])
    wada_d = din("w_ada", [D, 6 * D])
    bada_d = din("b_ada", [6 * D])
    n1g_d = din("norm1_g", [D])
    n2g_d = din("norm2_g", [D])
    fing_d = din("final_g", [D])
    win_d = din("w_in", [D, INC])
    mu_d = din("mu_fm", [128, 14])
    pf_d = din("pf", [128, 7, 4])
    w2_d = din("rw_w2", [64, 512])
    a2_d = din("rw_a2", [64, 512])
    g2_d = din("rw_g2", [128, 512])
    lng_d = din("gm_ln_g", [512])
    lnb_d = din("gm_ln_b", [512])
    wsT_d = din("wsT", [128, 8, 128])
    bs_d = din("bs_tm", [128, 8])
    wbr_rw_d = din("w_br_rwkv", [512, D])
    wbr_gm_d = din("w_br_gmlp", [512, D])
    wout_d = din("w_out", [D, D])
    rw_d = din("router_w", [D, NE])
    rb_d = din("router_b", [NE])
    MR = NE * 128 if stage >= 3 else 128
    mg_d = din("moe_wg", [MR, 2048])
    mup_d = din("moe_wu", [MR, 2048])
    md_d = din("moe_wd", [MR, 2048])
    sg_d = din("sh_w_gate", [D, 256])
    su_d = din("sh_w_up", [D, 256])
    sd_d = din("sh_w_down", [256, D])
    cst_d = din("cst", [128, 5, 128])
    misc_d = din("misc", [128, 4])
    iota_d = din("iota1", [NE])
    out_d = nc.dram_tensor("out", [S, D], F32, kind="ExternalOutput").ap()

    def dscr(name, shape, dt):
        return nc.dram_tensor(name, shape, dt, kind="Internal").ap()

    hT_d = dscr("hT_s", [NT, 128, 8 * 128], BF16); hT_db = Buf()
    orw_d = dscr("orw_s", [NT, 128, 512], BF16); orw_db = Buf()
    ogT_d = dscr("ogT_s", [NT, 128, 512], BF16); ogT_db = Buf()
    acc_d = dscr("acc_s", [S, D], F32); acc_db = Buf()
    h2_d = dscr("h2_s", [S, D], BF16); h2_db = Buf()
    xs_d = dscr("xs_s", [NBLK * 128, D], BF16); xs_db = Buf()
    ob_d = dscr("ob_s", [NBLK * 128, D], F32); ob_db = Buf()
    outb = Buf()
    dbg_d = nc.dram_tensor("dbg", [S, D], F32, kind="ExternalOutput").ap() if dbg else None
    dbgb = Buf()

    PH = Arena(nc, st, "arena", 207 * 1024)

    class _Pers:
        def alloc(self, name, shape, dt=F32):
            return PH.alloc(name, shape, dt, pers=True)
    PERS = _Pers()

    def DMA(eng, out, in_, R, W):
        P.add(eng, lambda e: e.dma_start(out=out, in_=in_), reads=R, writes=W, dma=True)

    def MM(out, lhsT, rhs, R, W, start=True, stop=True):
        P.add("tensor", lambda e: e.matmul(out, lhsT=lhsT, rhs=rhs, start=start, stop=stop), reads=R, writes=W)

    def TR(out, in_, ident, R, W):
        P.add("tensor", lambda e: e.transpose(out, in_, ident), reads=R, writes=W)

    def ACT(out, in_, func, R, W, **kw):
        P.add("scalar", lambda e: e.activation(out=out, in_=in_, func=func, **kw), reads=R, writes=W)

    def TT(eng, out, in0, in1, op, R, W):
        P.add(eng, lambda e: e.tensor_tensor(out=out, in0=in0, in1=in1, op=op), reads=R, writes=W)

    def TS(eng, out, in0, s1, s2, op0, op1, R, W):
        if op1 is None:
            P.add(eng, lambda e: e.tensor_scalar(out=out, in0=in0, scalar1=s1, scalar2=None, op0=op0), reads=R, writes=W)
        else:
            P.add(eng, lambda e: e.tensor_scalar(out=out, in0=in0, scalar1=s1, scalar2=s2, op0=op0, op1=op1), reads=R, writes=W)

    def STT(eng, out, in0, scalar, in1, op0, op1, R, W):
        P.add("vector", lambda e: e.scalar_tensor_tensor(out=out, in0=in0, scalar=scalar, in1=in1, op0=op0, op1=op1), reads=R, writes=W)

    def CP(eng, out, in_, R, W):
        if eng == "scalar":
            P.add(eng, lambda e: e.copy(out=out, in_=in_), reads=R, writes=W)
        else:
            P.add(eng, lambda e: e.tensor_copy(out=out, in_=in_), reads=R, writes=W)

    def RSUM(eng, out, in_, R, W):
        P.add(eng, lambda e: e.reduce_sum(out=out, in_=in_, axis=AX.X), reads=R, writes=W)

    def MAX8(out, in_, R, W):
        P.add("vector", lambda e: e.max(out=out, in_=in_), reads=R, writes=W)

    def MSET(eng, ap, val, W):
        P.add(eng, lambda e: e.memset(ap, val), writes=W)

    def barrier():
        P.barrier()

    ps = st.enter_context(nc.psum_tensor("ps", [128, 8, 512], F32))
    psb = [Buf("psb%d" % i) for i in range(8)]
    rot = [0]

    def bank():
        i = rot[0]
        rot[0] = (i + 1) % 8
        return i

    def pf32(i):
        return ps[:, i, :]

    def pbf(i):
        return ps[:, i, :].bitcast(BF16)

    def RSQ(out, in_, buf):
        ACT(out, in_, AF.Sqrt, [buf], [buf])
        P.add("vector", lambda e: e.reciprocal(out=out, in_=out), reads=[buf], writes=[buf])

    def rmsnorm_rstd(src, sqf, ss, eps=1e-6):
        ACT(sqf[:], src[:], AF.Square, [src.b], [sqf.b])
        RSUM("vector", ss[:, 0:1], sqf[:], [sqf.b], [ss.b])
        TS("vector", ss[:, 1:2], ss[:, 0:1], 1.0 / D, eps, ALU.mult, ALU.add, [ss.b], [ss.b])
        RSQ(ss[:, 2:3], ss[:, 1:2], ss.b)

    def transpose8(src_bf, dst, n=8):
        bk = bank()
        for kc in range(n):
            TR(pbf(bk)[:, kc * 128:(kc + 1) * 128], src_bf[:, kc * 128:(kc + 1) * 128], ident_b, [src_bf.b, cst_b.b], [psb[bk]])
        CP("scalar", dst[:].rearrange("p a b -> p (a b)"), pbf(bk)[:, 0:n * 128], [psb[bk]], [dst.b])

    cst_f = PERS.alloc("cst_f", [128, 5, 128])
    cst_b = PERS.alloc("cst_b", [128, 5, 128], BF16)
    DMA("sync", cst_f[:], cst_d, [], [cst_f.b])
    DMA("gpsimd", cst_b[:], cst_d, [], [cst_b.b])
    ident_b = cst_b[:, 0, :]
    blk_f = cst_f[:, 4, :]
    blk_b = cst_b[:, 4, :]
    ones_f = PERS.alloc("ones_f", [128, 256])
    MSET("vector", ones_f[:], 1.0, [ones_f.b])
    ones_b = PERS.alloc("ones_b", [128, 128], BF16)
    MSET("vector", ones_b[:], 1.0, [ones_b.b])
    misc = PERS.alloc("misc", [128, 4])
    DMA("sync", misc[:], misc_d, [], [misc.b])
    modr = PERS.alloc("modr", [128, 6, D])
    gs1 = PERS.alloc("gs1", [128, D])
    gs2 = PERS.alloc("gs2", [128, D])

    ct = PH.alloc("ct", [128, 8])
    sct = PH.alloc("sct", [128, 8])
    screp = PH.alloc("screp", [128, 8, 128], BF16)
    wst = [PH.alloc("wada%d" % i, [128, 8, 512]) for i in range(2)]
    wstb = [PH.alloc("wadab%d" % i, [128, 8, 512], BF16) for i in range(2)]
    ng = PH.alloc("ng", [128, 2, D])
    DMA("sync", ct[:], c_d, [], [ct.b])
    DMA("sync", modr[:].rearrange("p a d -> p (a d)"), bada_d.partition_broadcast(128), [], [modr.b])
    DMA("sync", ng[:, 0, :], n1g_d.partition_broadcast(128), [], [ng.b])
    DMA("sync", ng[:, 1, :], n2g_d.partition_broadcast(128), [], [ng.b])
    ACT(sct[:], ct[:], AF.Silu, [ct.b], [sct.b])
    for kc in range(8):
        ACT(screp[:, kc, :], ones_f[:, 0:128], AF.Copy, [ones_f.b, sct.b], [screp.b], scale=sct[:, kc:kc + 1])
    wv = wada_d.rearrange("(kc p) n -> p kc n", p=128)
    for j in range(12):
        w = wst[j % 2]
        DMA("sync", w[:], wv[:, :, j * 512:(j + 1) * 512], [], [w.b])
        wb = wstb[j % 2]
        CP("vector" if j % 2 else "scalar", wb[:], w[:], [w.b], [wb.b])
        bk = bank()
        for kc in range(8):
            MM(pf32(bk), screp[:, kc, :], wb[:, kc, :], [screp.b, wb.b], [psb[bk]], start=(kc == 0), stop=(kc == 7))
        a, hlf = j // 2, j % 2
        TT("vector", modr[:, a, hlf * 512:(hlf + 1) * 512], pf32(bk), modr[:, a, hlf * 512:(hlf + 1) * 512], ALU.add,
           [psb[bk], modr.b], [modr.b])
    STT("vector", gs1[:], modr[:, 1, :], 1.0, ng[:, 0, :], ALU.add, ALU.mult, [modr.b, ng.b], [gs1.b])
    STT("vector", gs2[:], modr[:, 4, :], 1.0, ng[:, 1, :], ALU.add, ALU.mult, [modr.b, ng.b], [gs2.b])
    sh1, gt1, sh2, gt2 = modr[:, 0, :], modr[:, 2, :], modr[:, 3, :], modr[:, 5, :]

    barrier()
    PH.reset()
    winA = PH.alloc("winA", [128, 8, 2816], BF16)
    winv = win_d.rearrange("(kc p) n -> p kc n", p=128)
    for kc in range(8):
        DMA("gpsimd", winA[:, kc, :], winv[:, kc, 0:2816], [], [winA.b])
    w2_b = PH.alloc("w2_b", [128, 512], BF16)
    a2_b = PH.alloc("a2_b", [128, 512], BF16)
    g2_b = PH.alloc("g2_b", [128, 512], BF16)
    DMA("gpsimd", w2_b[0:64, :], w2_d, [], [w2_b.b])
    DMA("gpsimd", a2_b[64:128, :], a2_d, [], [a2_b.b])
    DMA("gpsimd", g2_b[:], g2_d, [], [g2_b.b])
    wsT_b = PH.alloc("wsT_b", [128, 8, 128], BF16)
    DMA("gpsimd", wsT_b[:], wsT_d, [], [wsT_b.b])
    TT("gpsimd", wsT_b[:], wsT_b[:], cst_b[:, 2:3, :].to_broadcast([128, 8, 128]), ALU.mult, [wsT_b.b, cst_b.b], [wsT_b.b])
    mu = PH.alloc("mu", [128, 14])
    pfm = PH.alloc("pfm", [128, 7, 4])
    omka = PH.alloc("omka", [128, 4])
    DMA("sync", mu[:], mu_d, [], [mu.b])
    DMA("sync", pfm[:], pf_d, [], [pfm.b])
    TS("vector", omka[:], pfm[:, 3, :], -1.0, 1.0, ALU.mult, ALU.add, [pfm.b], [omka.b])
    lng = PH.alloc("lng", [128, 512])
    lnb = PH.alloc("lnb", [128, 512])
    bs_tm = PH.alloc("bs_tm", [128, 8])
    DMA("sync", lng[:], lng_d.partition_broadcast(128), [], [lng.b])
    DMA("sync", lnb[:], lnb_d.partition_broadcast(128), [], [lnb.b])
    DMA("sync", bs_tm[:], bs_d, [], [bs_tm.b])
    XTs = PH.alloc("xt", [128, D])
    sqf = Tl(modr[:, 1, :], "sqf")
    y_f = Tl(modr[:, 4, 0:512].rearrange("p (a b) -> p a b", a=4), "y_f")
    BVx = Tl(modr[:, 4, 512:1024].rearrange("p (a b) -> p a b", a=4), "BVx")
    ssn = PH.alloc("ssn", [128, 4])
    h_tm = PH.alloc("h_tm", [128, D], BF16)
    hT = PH.alloc("hT", [128, 8, 128], BF16)
    Praw = PH.alloc("Praw", [128, 14, 129])
    XS = PH.alloc("XS", [128, 14, 128])
    MSET("gpsimd", Praw[:, :, 0:1], 0.0, [Praw.b])
    tw = PH.alloc("tw", [128, 128], BF16)
    xa_b = PH.alloc("xa_b", [128, 128], BF16)
    sg_b = PH.alloc("sg_b", [128, 128], BF16)

    def F4(name):
        return PH.alloc(name, [128, 4, 128])

    def B4(name):
        return PH.alloc(name, [128, 4, 128], BF16)
    sgz, cum, Pt, iP, Pm1, alr, KK, rn, tmpk, kmod = [F4(n) for n in
        ("sgz", "cum", "Pt", "iP", "Pm1", "alr", "KK", "rn", "tmpk", "kmod")]
    sq_b, Bt, Kt, vb, orw_b = [B4(n) for n in ("sq_b", "Bt", "Kt", "vb", "orw_b")]
    rkb = sq_b
    IF = []
    for i in range(2):
        IF.append(dict(
            AR=PH.alloc("AR%d" % i, [128, 4, 2, 128], BF16), Vtm=PH.alloc("Vtm%d" % i, [128, 4, 128], BF16),
            BtT=PH.alloc("BtT%d" % i, [128, 4, 128], BF16), KtT=PH.alloc("KtT%d" % i, [128, 4, 128], BF16),
            XA=PH.alloc("XA%d" % i, [128, 8, 2, 128], BF16), KA2=PH.alloc("KA2%d" % i, [128, 8, 2, 128], BF16),
            XT0=PH.alloc("XT0%d" % i, [128, 8, 128], BF16), g_f=F4("g_f%d" % i), PL=PH.alloc("PL%d" % i, [128, 4]),
            BV=(F4("BV0") if i == 0 else BVx)))
    Xp = [PH.alloc("Xp%d" % i, [128, 8, 128], BF16) for i in range(2)]
    XTp = [PH.alloc("XTp%d" % i, [128, 8, 128], BF16) for i in range(2)]
    Mf = PH.alloc("Mf", [128, 8, 128])
    Mb = PH.alloc("Mb", [128, 8, 128], BF16)
    for _t in Xp + XTp + [Mf, Mb]:
        _t.bq = [Buf(_t.b.name + "q0"), Buf(_t.b.name + "q1")]
    yd = Tl(Mf[:, 0:4, :], "yd")
    yd.b = Mf.bq[0]
    yd2 = Tl(Mf[:, 4:8, :], "yd2")
    yd2.b = Mf.bq[1]
    W0T_b = PH.alloc("W0T_b", [128, 512], BF16)
    UT_b = PH.alloc("UT_b", [128, 512], BF16)
    ST_f = PH.alloc("ST_f", [128, 4, 64])
    ST_b = PH.alloc("ST_b", [128, 4, 64], BF16)
    MSET("gpsimd", ST_f[:], 0.0, [ST_f.b])
    MSET("gpsimd", ST_b[:], 0.0, [ST_b.b])
    ZU = [PH.alloc("zu%d" % i, [128, 512]) for i in range(2)]
    ZV = [PH.alloc("zv%d" % i, [128, 8, 64]) for i in range(2)]
    dv = PH.alloc("dv", [128, 8, 64])
    dv2 = PH.alloc("dv2", [128, 8, 64])
    gst = PH.alloc("gst", [128, 4, 8])
    vn_b = PH.alloc("vn_b", [128, 512], BF16)
    ogm_b = vn_b
    ogT_b = PH.alloc("ogT_b", [128, 4, 128], BF16)
    msk_2 = cst_f[:, 1:3, :]
    msk_l = cst_f[:, 3, :]

    EXPC = 0.6065306597126334
    DMA("sync", XTs[:], x_d[0:128, :], [], [XTs.b])

    def front(ti):
        zu, zv = ZU[ti % 2], ZV[ti % 2]
        xt = XTs
        rmsnorm_rstd(xt, sqf, ssn)
        STT("vector", sqf[:], xt[:], ssn[:, 2:3], gs1[:], ALU.mult, ALU.mult, [xt.b, ssn.b, gs1.b, sqf.b], [sqf.b])
        if ti + 1 < NT:
            DMA("sync", XTs[:], x_d[(ti + 1) * 128:(ti + 2) * 128, :], [], [XTs.b])
        TT("vector", h_tm[:], sqf[:], sh1, ALU.add, [sqf.b, modr.b], [h_tm.b])
        yield
        transpose8(h_tm, hT)
        DMA("sync", hT_d[ti], hT[:].rearrange("p a b -> p (a b)"), [hT.b], [hT_db])
        yield
        for lo, hi in ((0, 4), (4, 8), (8, 12), (12, 14)):
            bk = bank()
            for f in range(lo, hi):
                for kc in range(8):
                    MM(pf32(bk)[:, (f - lo) * 128:(f - lo + 1) * 128], winA[:, kc, f * 128:(f + 1) * 128], hT[:, kc, :],
                       [winA.b, hT.b], [psb[bk]], start=(kc == 0), stop=(kc == 7))
            CP("scalar", Praw[:, lo:hi, 1:129], pf32(bk)[:, 0:(hi - lo) * 128].rearrange("p (a b) -> p a b", a=hi - lo),
               [psb[bk]], [Praw.b])
            yield
        for half, dst in ((0, zu[:]), (1, zv[:].rearrange("p a b -> p (a b)"))):
            bk = bank()
            for kc in range(8):
                MM(pf32(bk), hT[:, kc, :], winA[:, kc, RWC + half * 512:RWC + (half + 1) * 512], [winA.b, hT.b], [psb[bk]],
                   start=(kc == 0), stop=(kc == 7))
            ACT(dst, pf32(bk), AF.Gelu_apprx_tanh, [psb[bk]], [zu.b if half == 0 else zv.b])
            yield

    def prep(ti):
        I = IF[ti % 2]
        AR, Vtm, BtT, KtT, XA, KA2, XT0, g_f, PL, BV = (I[k] for k in ("AR", "Vtm", "BtT", "KtT", "XA", "KA2", "XT0", "g_f", "PL", "BV"))
        TT("vector", XS[:], Praw[:, :, 0:128], Praw[:, :, 1:129], ALU.subtract, [Praw.b], [XS.b])
        for f in range(14):
            STT("vector", XS[:, f, :], XS[:, f, :], mu[:, f:f + 1], Praw[:, f, 1:129], ALU.mult, ALU.add,
                [XS.b, mu.b, Praw.b], [XS.b])
        CP("gpsimd", Praw[:, :, 0:1], Praw[:, :, 128:129], [Praw.b, XS.b], [Praw.b])
        yield
        ACT(tw[0:64, :], XS[0:64, 12, :], AF.Tanh, [XS.b], [tw.b])
        CP("scalar", xa_b[64:128, :], XS[64:128, 12, :], [XS.b], [xa_b.b])
        ACT(sg_b[:], XS[:, 13, :], AF.Sigmoid, [XS.b], [sg_b.b])
        yield
        bz, ba, bg = bank(), bank(), bank()
        for f in range(4):
            MM(pf32(bz)[:, f * 128:(f + 1) * 128], w2_b[0:64, f * 128:(f + 1) * 128], tw[0:64, :], [w2_b.b, tw.b], [psb[bz]])
        for f in range(4):
            MM(pf32(ba)[:, f * 128:(f + 1) * 128], a2_b[64:128, f * 128:(f + 1) * 128], xa_b[64:128, :], [a2_b.b, xa_b.b], [psb[ba]])
        for f in range(4):
            MM(pf32(bg)[:, f * 128:(f + 1) * 128], g2_b[:, f * 128:(f + 1) * 128], sg_b[:], [g2_b.b, sg_b.b], [psb[bg]])
        for f in range(4):
            ACT(sgz[:, f, :], pf32(bz)[:, f * 128:(f + 1) * 128], AF.Sigmoid, [psb[bz], pfm.b], [sgz.b], bias=pfm[:, 0, f:f + 1])
        for f in range(4):
            ACT(alr[:, f, :], pf32(ba)[:, f * 128:(f + 1) * 128], AF.Sigmoid, [psb[ba], pfm.b], [alr.b], bias=pfm[:, 1, f:f + 1])
        CP("vector", g_f[:].rearrange("p a b -> p (a b)"), pf32(bg), [psb[bg]], [g_f.b])
        yield
        for f in range(4):
            P.add("vector", (lambda o, d1: (lambda e: e.tensor_tensor_scan(out=o, data0=ones_f[:, 0:128], data1=d1, initial=0.0,
                                                                             op0=ALU.mult, op1=ALU.add)))(cum[:, f, :], sgz[:, f, :]),
                  reads=[sgz.b, ones_f.b], writes=[cum.b])
        ACT(Pt[:], cum[:], AF.Exp, [cum.b], [Pt.b], scale=-EXPC)
        ACT(iP[:], cum[:], AF.Exp, [cum.b], [iP.b], scale=EXPC)
        TT("gpsimd", tmpk[:], cum[:], sgz[:], ALU.subtract, [cum.b, sgz.b], [tmpk.b])
        ACT(Pm1[:], tmpk[:], AF.Exp, [tmpk.b], [Pm1.b], scale=-EXPC)
        CP("gpsimd", PL[:], Pt[:, :, 127], [Pt.b], [PL.b])
        yield
        for f in range(4):
            ACT(KK[:, f, :], XS[:, 4 + f, :], AF.Copy, [XS.b, pfm.b], [KK.b], scale=pfm[:, 2, f:f + 1])
        ACT(sq_b[:], KK[:], AF.Square, [KK.b], [sq_b.b])
        bk = bank()
        MM(pf32(bk), blk_b, sq_b[:].rearrange("p a b -> p (a b)"), [cst_b.b, sq_b.b], [psb[bk]])
        TS("vector", rn[:].rearrange("p a b -> p (a b)"), pf32(bk), 1e-24, None, ALU.max, None, [psb[bk]], [rn.b])
        RSQ(rn[:], rn[:], rn.b)
        yield
        TT("vector", KK[:], KK[:], rn[:], ALU.mult, [KK.b, rn.b], [KK.b])
        for f in range(4):
            ACT(tmpk[:, f, :], alr[:, f, :], AF.Identity, [alr.b, pfm.b, omka.b, tmpk.b], [tmpk.b],
                scale=pfm[:, 3, f:f + 1], bias=omka[:, f:f + 1])
        TT("vector", kmod[:], XS[:, 4:8, :], tmpk[:], ALU.mult, [XS.b, tmpk.b], [kmod.b])
        TT("gpsimd", AR[:, :, 1, :], XS[:, 0:4, :], Pt[:], ALU.mult, [XS.b, Pt.b], [AR.b])
        STT("vector", AR[:, :, 0, :], KK[:], -1.0, Pm1[:], ALU.mult, ALU.mult, [KK.b, Pm1.b, AR.b], [AR.b])
        yield
        TT("vector", rn[:], KK[:], alr[:], ALU.mult, [KK.b, alr.b, rn.b], [rn.b])
        TT("vector", Bt[:], rn[:], iP[:], ALU.mult, [rn.b, iP.b], [Bt.b])
        TT("vector", Kt[:], kmod[:], iP[:], ALU.mult, [kmod.b, iP.b], [Kt.b])
        CP("gpsimd", vb[:], XS[:, 8:12, :], [XS.b], [vb.b])
        yield
        for f in range(4):
            STT("vector", rkb[:, f, :], XS[:, f, :], pfm[:, 4, f:f + 1], kmod[:, f, :], ALU.mult, ALU.mult,
                [XS.b, pfm.b, kmod.b], [rkb.b])
        bk = bank()
        MM(pf32(bk), blk_b, rkb[:].rearrange("p a b -> p (a b)"), [cst_b.b, rkb.b], [psb[bk]])
        TT("vector", BV[:].rearrange("p a b -> p (a b)"), pf32(bk), XS[:, 8:12, :].rearrange("p a b -> p (a b)"), ALU.mult,
           [psb[bk], XS.b], [BV.b])
        yield
        for src, dst in ((vb, Vtm), (Bt, BtT), (Kt, KtT)):
            bk = bank()
            for f in range(4):
                TR(pbf(bk)[:, f * 128:(f + 1) * 128], src[:, f, :], ident_b, [src.b, cst_b.b], [psb[bk]])
            CP("scalar", dst[:].rearrange("p a b -> p (a b)"), pbf(bk)[:, 0:512], [psb[bk]], [dst.b])
            yield
        for f in range(4):
            for hp in range(2):
                b1, b2, b3 = bank(), bank(), bank()
                pb = hp * 64
                h = 2 * f + hp
                arh = AR[pb:pb + 64, f, :, :].rearrange("p a b -> p (a b)")
                MM(pf32(b1)[:, 0:256], Bt[pb:pb + 64, f, :], arh, [Bt.b, AR.b], [psb[b1]])
                MM(pf32(b2)[:, 0:256], Kt[pb:pb + 64, f, :], arh, [Kt.b, AR.b], [psb[b2]])
                MM(pf32(b3)[:, 0:128], AR[pb:pb + 64, f, 0, :], Bt[pb:pb + 64, f, :], [Bt.b, AR.b], [psb[b3]])
                TT("vector", XA[:, h, :, :], pf32(b1)[:, 0:256].rearrange("p (a b) -> p a b", a=2), msk_2, ALU.mult,
                   [psb[b1], cst_f.b], [XA.b])
                TT("vector", KA2[:, h, :, :], pf32(b2)[:, 0:256].rearrange("p (a b) -> p a b", a=2), msk_2, ALU.mult,
                   [psb[b2], cst_f.b], [KA2.b])
                TT("vector", XT0[:, h, :], pf32(b3)[:, 0:128], msk_l, ALU.mult,
                   [psb[b3], cst_f.b], [XT0.b])
            yield

    def chain(ti):
        I = IF[ti % 2]
        AR, Vtm, BtT, KtT, XA, KA2, XT0, PL = (I[k] for k in ("AR", "Vtm", "BtT", "KtT", "XA", "KA2", "XT0", "PL"))
        TT("vector", Mb[:], XA[:, :, 0, :], cst_f[:, 0:1, :].to_broadcast([128, 8, 128]), ALU.add, [XA.b, cst_f.b], [Mb.bq[0], Mb.bq[1]])

        def views(t, strided):
            return (lambda h: t[:, h, 0, :]) if strided else (lambda h: t[:, h, :])
        Xc, XTc, Xcb, XTcb = views(XA, True), views(XT0, False), [XA.b, XA.b], [XT0.b, XT0.b]
        for stp in range(7):
            do_prod = stp >= 1
            do_sq = stp <= 5
            bm, bx, bxt = [None, None], [None, None], [None, None]
            for q in range(2):
                hs = range(4 * q, 4 * q + 4)
                if do_prod:
                    bm[q] = bank()
                    for h in hs:
                        MM(pf32(bm[q])[:, (h % 4) * 128:(h % 4 + 1) * 128], XTc(h), Mb[:, h, :], [XTcb[q], Mb.bq[q]], [psb[bm[q]]])
                if do_sq:
                    bx[q], bxt[q] = bank(), bank()
                    for h in hs:
                        MM(pf32(bx[q])[:, (h % 4) * 128:(h % 4 + 1) * 128], XTc(h), Xc(h), [XTcb[q], Xcb[q]], [psb[bx[q]]])
                    for h in hs:
                        MM(pf32(bxt[q])[:, (h % 4) * 128:(h % 4 + 1) * 128], Xc(h), XTc(h), [XTcb[q], Xcb[q]], [psb[bxt[q]]])
            nx, nxt = Xp[stp % 2], XTp[stp % 2]
            for q in range(2):
                sl = slice(4 * q, 4 * q + 4)
                if do_prod:
                    TT("vector", Mb[:, sl, :], pf32(bm[q]).rearrange("p (a b) -> p a b", a=4), Mb[:, sl, :],
                       ALU.add, [psb[bm[q]], Mb.bq[q]], [Mb.bq[q]])
                if do_sq:
                    CP("scalar", nx[:, sl, :], pf32(bx[q]).rearrange("p (a b) -> p a b", a=4), [psb[bx[q]]], [nx.bq[q]])
                    CP("scalar", nxt[:, sl, :], pf32(bxt[q]).rearrange("p (a b) -> p a b", a=4), [psb[bxt[q]]], [nxt.bq[q]])
            if do_sq:
                Xc, XTc, Xcb, XTcb = views(nx, False), views(nxt, False), nx.bq, nxt.bq
            yield
        bW = bank()
        for h in range(8):
            f, pb = h // 2, (h % 2) * 64
            MM(pf32(bW)[:, h * 64:(h + 1) * 64], AR[pb:pb + 64, f, 0, :], ST_b[pb:pb + 64, f, :], [AR.b, ST_b.b], [psb[bW]],
               start=True, stop=False)
            MM(pf32(bW)[:, h * 64:(h + 1) * 64], KA2[:, h, 0, :], Vtm[:, f, (h % 2) * 64:(h % 2) * 64 + 64], [KA2.b, Vtm.b], [psb[bW]],
               start=False, stop=True)
        CP("scalar", W0T_b[:], pf32(bW), [psb[bW]], [W0T_b.b])
        yield
        bU = bank()
        for h in range(8):
            MM(pf32(bU)[:, h * 64:(h + 1) * 64], Mb[:, h, :], W0T_b[:, h * 64:(h + 1) * 64], [Mb.bq[h // 4], W0T_b.b], [psb[bU]])
        CP("vector", UT_b[:], pf32(bU), [psb[bU]], [UT_b.b])
        yield
        bY, bS = bank(), bank()
        for h in range(8):
            f, pb = h // 2, (h % 2) * 64
            hc = slice((h % 2) * 64, (h % 2) * 64 + 64)
            oy = ps[pb:pb + 64, bY, f * 128:(f + 1) * 128]
            MM(oy, ST_b[pb:pb + 64, f, :], AR[pb:pb + 64, f, 1, :], [ST_b.b, AR.b], [psb[bY]], start=True, stop=False)
            MM(oy, UT_b[:, h * 64:(h + 1) * 64], XA[:, h, 1, :], [UT_b.b, XA.b], [psb[bY]], start=False, stop=False)
            MM(oy, Vtm[:, f, hc], KA2[:, h, 1, :], [Vtm.b, KA2.b], [psb[bY]], start=False, stop=True)
        for h in range(8):
            f, pb = h // 2, (h % 2) * 64
            hc = slice((h % 2) * 64, (h % 2) * 64 + 64)
            os_ = ps[pb:pb + 64, bS, f * 64:(f + 1) * 64]
            MM(os_, BtT[:, f, hc], UT_b[:, h * 64:(h + 1) * 64], [BtT.b, UT_b.b], [psb[bS]], start=True, stop=False)
            MM(os_, KtT[:, f, hc], Vtm[:, f, hc], [KtT.b, Vtm.b], [psb[bS]], start=False, stop=True)
        CP("vector", y_f[:].rearrange("p a b -> p (a b)"), pf32(bY), [psb[bY]], [y_f.b])
        for f in range(4):
            ACT(ST_f[:, f, :], ST_f[:, f, :], AF.Copy, [ST_f.b, PL.b, ST_b.b], [ST_f.b], scale=PL[:, f:f + 1])
        for f in range(4):
            STT("vector", ST_f[:, f, :], ps[:, bS, f * 64:(f + 1) * 64], PL[:, f:f + 1], ST_f[:, f, :], ALU.mult, ALU.add,
                [psb[bS], PL.b, ST_f.b], [ST_f.b])
        CP("gpsimd", ST_b[:], ST_f[:], [ST_f.b], [ST_b.b])
        yield

    def gn(ti):
        I = IF[ti % 2]
        g_f, BV = I["g_f"], I["BV"]
        bk = bank()
        MM(pf32(bk), blk_f, y_f[:].rearrange("p a b -> p (a b)"), [cst_f.b, y_f.b], [psb[bk]])
        STT("vector", yd[:].rearrange("p a b -> p (a b)"), pf32(bk), -1.0 / 64, y_f[:].rearrange("p a b -> p (a b)"),
            ALU.mult, ALU.add, [psb[bk], y_f.b, yd.b], [yd.b])
        yield
        ACT(yd2[:], yd[:], AF.Square, [yd.b, yd2.b], [yd2.b])
        yield
        bk = bank()
        MM(pf32(bk), blk_f, yd2[:].rearrange("p a b -> p (a b)"), [cst_f.b, yd2.b], [psb[bk]])
        TS("vector", yd2[:].rearrange("p a b -> p (a b)"), pf32(bk), 1.0 / 64, 64e-5, ALU.mult, ALU.add,
           [psb[bk], yd2.b], [yd2.b])
        yield
        RSQ(yd2[:], yd2[:], yd2.b)
        yield
        TT("vector", yd[:], yd[:], yd2[:], ALU.mult, [yd.b, yd2.b], [yd.b])
        for f in range(4):
            ACT(yd[:, f, :], yd[:, f, :], AF.Identity, [yd.b, pfm.b], [yd.b], scale=pfm[:, 5, f:f + 1], bias=pfm[:, 6, f:f + 1])
        yield
        TT("vector", yd[:], yd[:], BV[:], ALU.add, [yd.b, BV.b], [yd.b])
        TT("vector", orw_b[:], yd[:], g_f[:], ALU.mult, [yd.b, g_f.b], [orw_b.b])
        DMA("sync", orw_d[ti], orw_b[:].rearrange("p a b -> p (a b)"), [orw_b.b], [orw_db])
        if dbg and stage == 1:
            DMA("sync", dbg_d[ti * 128:(ti + 1) * 128, 0:512], y_f[:].rearrange("p a b -> p (a b)"), [y_f.b], [dbgb])
        yield

    def gm(ti):
        zu, zv = ZU[ti % 2], ZV[ti % 2]
        dvf = dv[:].rearrange("p a b -> p (a b)")
        RSUM("vector", gst[:, 0, :], zv[:], [zv.b], [gst.b])
        TS("vector", gst[:, 1, :], gst[:, 0, :], -1.0 / 64, None, ALU.mult, None, [gst.b], [gst.b])
        TT("gpsimd", dv[:], zv[:], gst[:, 1, :].unsqueeze(2).to_broadcast([128, 8, 64]), ALU.add, [zv.b, gst.b], [dv.b])
        yield
        ACT(dv2[:], dv[:], AF.Square, [dv.b], [dv2.b])
        RSUM("vector", gst[:, 2, :], dv2[:], [dv2.b], [gst.b])
        TS("vector", gst[:, 3, :], gst[:, 2, :], 1.0 / 64, 1e-5, ALU.mult, ALU.add, [gst.b], [gst.b])
        RSQ(gst[:, 3, :], gst[:, 3, :], gst.b)
        yield
        TT("gpsimd", dv[:], dv[:], gst[:, 3, :].unsqueeze(2).to_broadcast([128, 8, 64]), ALU.mult, [dv.b, gst.b], [dv.b])
        TT("gpsimd", dvf, dvf, lng[:], ALU.mult, [dv.b, lng.b], [dv.b])
        TT("gpsimd", vn_b[:], dvf, lnb[:], ALU.add, [dv.b, lnb.b], [vn_b.b])
        yield
        bk = bank()
        for g in range(8):
            MM(pf32(bk)[:, g * 64:(g + 1) * 64], wsT_b[:, g, :], vn_b[:, g * 64:(g + 1) * 64], [wsT_b.b, vn_b.b], [psb[bk]])
        TT("vector", dv2[:], pf32(bk).rearrange("p (a b) -> p a b", a=8), bs_tm[:].unsqueeze(2).to_broadcast([128, 8, 64]), ALU.add,
           [psb[bk], bs_tm.b, dv2.b], [dv2.b])
        yield
        TT("gpsimd", ogm_b[:], dv2[:].rearrange("p a b -> p (a b)"), zu[:], ALU.mult, [dv2.b, zu.b, ogm_b.b], [ogm_b.b])
        transpose8(ogm_b, ogT_b, n=4)
        DMA("sync", ogT_d[ti], ogT_b[:].rearrange("p a b -> p (a b)"), [ogT_b.b], [ogT_db])
        if dbg and stage == 1:
            DMA("sync", dbg_d[ti * 128:(ti + 1) * 128, 512:1024], zu[:], [zu.b], [dbgb])
        yield

    def interleave(*gens):
        gens = [g for g in gens if g is not None]
        while gens:
            for g in list(gens):
                try:
                    next(g)
                except StopIteration:
                    gens.remove(g)

    interleave(front(0))
    interleave(prep(0), gm(0))
    if NT > 1:
        interleave(front(1))
    for ti in range(NT):
        interleave(chain(ti), prep(ti + 1) if ti + 1 < NT else None, gm(ti + 1) if ti + 1 < NT else None)
        interleave(gn(ti), front(ti + 2) if ti + 2 < NT else None)

    if stage == 1:
        P.emit(nc)
        return nc, st

    barrier()
    PH.reset()
    wts_all = PERS.alloc("wts_all", [128, NT, NE])
    d8_all = PERS.alloc("d8_all", [128, NT, 8], I32)
    w8_all = PERS.alloc("w8_all", [128, NT, 8])
    widx = PERS.alloc("widx", [128, NQ * 128], I32)
    winG = PH.alloc("winG", [128, 8, 2048], BF16)
    for kc in range(8):
        DMA("gpsimd", winG[:, kc, :], winv[:, kc, 2816:INC], [], [winG.b])
    wbr_rw = PH.alloc("wbr_rw", [128, 4, D], BF16)
    wbr_gm = PH.alloc("wbr_gm", [128, 4, D], BF16)
    wout_b = PH.alloc("wout_b", [128, 8, D], BF16)
    DMA("gpsimd", wbr_rw[:], wbr_rw_d.rearrange("(kc p) n -> p kc n", p=128), [], [wbr_rw.b])
    DMA("gpsimd", wbr_gm[:], wbr_gm_d.rearrange("(kc p) n -> p kc n", p=128), [], [wbr_gm.b])
    DMA("gpsimd", wout_b[:], wout_d.rearrange("(kc p) n -> p kc n", p=128), [], [wout_b.b])
    rw_b = PH.alloc("rw_b", [128, 8, NE], BF16)
    shgu = PH.alloc("shgu", [128, 8, 512], BF16)
    shd_b = PH.alloc("shd_b", [128, 2, D], BF16)
    DMA("gpsimd", rw_b[:], rw_d.rearrange("(kc p) n -> p kc n", p=128), [], [rw_b.b])
    DMA("gpsimd", shgu[:, :, 0:256], sg_d.rearrange("(kc p) n -> p kc n", p=128), [], [shgu.b])
    DMA("gpsimd", shgu[:, :, 256:512], su_d.rearrange("(kc p) n -> p kc n", p=128), [], [shgu.b])
    DMA("gpsimd", shd_b[:], sd_d.rearrange("(kc p) n -> p kc n", p=128), [], [shd_b.b])
    rb_rep = PH.alloc("rb_rep", [128, NE])
    DMA("sync", rb_rep[:], rb_d.partition_broadcast(128), [], [rb_rep.b])
    XT2 = [PH.alloc("xt2_%d" % i, [128, D]) for i in range(2)]
    hT2 = [PH.alloc("hT2_%d" % i, [128, 8, 128], BF16) for i in range(2)]
    orw2 = [PH.alloc("orw2_%d" % i, [128, 4, 128], BF16) for i in range(2)]
    ogT2 = [PH.alloc("ogT2_%d" % i, [128, 4, 128], BF16) for i in range(2)]
    Gs = PH.alloc("Gs", [128, 16, 128])
    t1 = PH.alloc("t1", [128, 8, 128])
    t2 = PH.alloc("t2", [128, 8, 128])
    mT_b = PH.alloc("mT_b", [128, 8, 128], BF16)
    tmpo = PH.alloc("tmpo", [128, D])
    x1t = PH.alloc("x1t", [128, D])
    X1 = [x1t, Tl(gs1[:], "x1t_b")]
    tmpoR = Tl(modr[:, 0, :], "tmpoR")
    sqf2 = Tl(modr[:, 1, :], "sqf2")
    acc_t = Tl(modr[:, 4, :], "acc_t")
    ssn2 = PH.alloc("ssn2", [128, 4])
    h2_tm = PH.alloc("h2_tm", [128, D], BF16)
    h2T = PH.alloc("h2T", [128, 8, 128], BF16)
    sc = PH.alloc("sc", [128, NE])
    ssel = PH.alloc("ssel", [128, NE])
    sgl = PH.alloc("sgl", [128, 256])
    top8 = PH.alloc("top8", [128, 8])
    rsm = PH.alloc("rsm", [128, 2])
    actT = PH.alloc("actT", [128, 2, 128], BF16)

    def load_ii(tj):
        DMA("sync", hT2[tj % 2][:].rearrange("p a b -> p (a b)"), hT_d[tj], [hT_db], [hT2[tj % 2].b])
        DMA("sync", orw2[tj % 2][:].rearrange("p a b -> p (a b)"), orw_d[tj], [orw_db], [orw2[tj % 2].b])
        DMA("sync", ogT2[tj % 2][:].rearrange("p a b -> p (a b)"), ogT_d[tj], [ogT_db], [ogT2[tj % 2].b])
        DMA("sync", XT2[tj % 2][:], x_d[tj * 128:(tj + 1) * 128, :], [], [XT2[tj % 2].b])

    def Gpart(ti):
        xt, hTt, orw_t, ogT_t, x1 = XT2[ti % 2], hT2[ti % 2], orw2[ti % 2], ogT2[ti % 2], X1[ti % 2]
        for q in range(4):
            bk = bank()
            for f in range(4):
                ft = q * 4 + f
                for kc in range(8):
                    MM(pf32(bk)[:, f * 128:(f + 1) * 128], winG[:, kc, ft * 128:(ft + 1) * 128], hTt[:, kc, :], [winG.b, hTt.b],
                       [psb[bk]], start=(kc == 0), stop=(kc == 7))
            ACT(Gs[:, q * 4:(q + 1) * 4, :].rearrange("p a b -> p (a b)"), pf32(bk), AF.Sigmoid, [psb[bk]], [Gs.b])
            yield
        for wbr, src, dst, go in ((wbr_rw, orw_t, t1, 0), (wbr_gm, ogT_t, t2, 8)):
            for q in range(2):
                bk = bank()
                for f in range(4):
                    ft = q * 4 + f
                    for kc in range(4):
                        MM(pf32(bk)[:, f * 128:(f + 1) * 128], wbr[:, kc, ft * 128:(ft + 1) * 128], src[:, kc, :], [wbr.b, src.b],
                           [psb[bk]], start=(kc == 0), stop=(kc == 3))
                TT("vector", dst[:, q * 4:(q + 1) * 4, :].rearrange("p a b -> p (a b)"), pf32(bk),
                   Gs[:, go + q * 4:go + (q + 1) * 4, :].rearrange("p a b -> p (a b)"), ALU.mult, [psb[bk], Gs.b], [dst.b])
                yield
        TT("vector", mT_b[:, 0:4, :], t1[:, 0:4, :], t2[:, 0:4, :], ALU.add, [t1.b, t2.b], [mT_b.b])
        TT("vector", mT_b[:, 4:8, :], t1[:, 4:8, :], t2[:, 4:8, :], ALU.add, [t1.b, t2.b], [mT_b.b])
        for n in range(2):
            bk = bank()
            for kc in range(8):
                MM(pf32(bk), mT_b[:, kc, :], wout_b[:, kc, n * 512:(n + 1) * 512], [mT_b.b, wout_b.b], [psb[bk]],
                   start=(kc == 0), stop=(kc == 7))
            TT("vector", tmpo[:, n * 512:(n + 1) * 512], pf32(bk), gt1[:, n * 512:(n + 1) * 512], ALU.mult, [psb[bk], modr.b], [tmpo.b])
            if n == 0:
                yield
        TT("vector", x1[:], tmpo[:], xt[:], ALU.add, [tmpo.b, xt.b], [x1.b])
        if dbg and stage == 2:
            DMA("sync", dbg_d[ti * 128:(ti + 1) * 128, :], x1[:], [x1.b], [dbgb])
        yield

    def Rpart(ti):
        x1 = X1[ti % 2]
        rmsnorm_rstd(x1, sqf2, ssn2)
        STT("vector", sqf2[:], x1[:], ssn2[:, 2:3], gs2[:], ALU.mult, ALU.mult, [x1.b, ssn2.b, gs2.b, sqf2.b], [sqf2.b])
        TT("vector", h2_tm[:], sqf2[:], sh2, ALU.add, [sqf2.b, modr.b], [h2_tm.b])
        DMA("sync", h2_d[ti * 128:(ti + 1) * 128, :], h2_tm[:], [h2_tm.b], [h2_db])
        yield
        transpose8(h2_tm, h2T)
        yield
        bk = bank()
        for kc in range(8):
            MM(pf32(bk)[:, 0:NE], h2T[:, kc, :], rw_b[:, kc, :], [h2T.b, rw_b.b], [psb[bk]], start=(kc == 0), stop=(kc == 7))
        ACT(sc[:], pf32(bk)[:, 0:NE], AF.Sigmoid, [psb[bk]], [sc.b])
        yield
        TT("gpsimd", ssel[:], sc[:], rb_rep[:], ALU.add, [sc.b, rb_rep.b], [ssel.b])
        MAX8(top8[:], ssel[:], [ssel.b], [top8.b])
        TS("vector", ssel[:], ssel[:], top8[:, 7:8], None, ALU.is_ge, None, [ssel.b, top8.b], [ssel.b])
        TT("gpsimd", sc[:], sc[:], ssel[:], ALU.mult, [sc.b, ssel.b], [sc.b])
        RSUM("vector", rsm[:, 0:1], sc[:], [sc.b], [rsm.b])
        P.add("vector", lambda e: e.reciprocal(out=rsm[:, 1:2], in_=rsm[:, 0:1]), reads=[rsm.b], writes=[rsm.b])
        TS("vector", wts_all[:, ti, :], sc[:], rsm[:, 1:2], 2.5, ALU.mult, ALU.mult, [sc.b, rsm.b], [wts_all.b])
        yield
        bk = bank()
        for j in range(4):
            for kc in range(8):
                MM(pf32(bk)[:, j * 128:(j + 1) * 128], shgu[:, kc, j * 128:(j + 1) * 128], h2T[:, kc, :], [shgu.b, h2T.b], [psb[bk]],
                   start=(kc == 0), stop=(kc == 7))
        ACT(sgl[:], pf32(bk)[:, 0:256], AF.Silu, [psb[bk]], [sgl.b])
        TT("vector", actT[:].rearrange("p a b -> p (a b)"), pf32(bk)[:, 256:512], sgl[:], ALU.mult, [psb[bk], sgl.b], [actT.b])
        yield
        for n in range(2):
            bk = bank()
            for fc in range(2):
                MM(pf32(bk), actT[:, fc, :], shd_b[:, fc, n * 512:(n + 1) * 512], [actT.b, shd_b.b], [psb[bk]],
                   start=(fc == 0), stop=(fc == 1))
            TT("vector", tmpoR[:, n * 512:(n + 1) * 512], pf32(bk), gt2[:, n * 512:(n + 1) * 512], ALU.mult,
               [psb[bk], modr.b, tmpoR.b], [tmpoR.b])
            if n == 0:
                yield
        TT("gpsimd", acc_t[:], tmpoR[:], x1[:], ALU.add, [tmpoR.b, x1.b], [acc_t.b])
        DMA("sync", acc_d[ti * 128:(ti + 1) * 128, :], acc_t[:], [acc_t.b], [acc_db])
        yield

    def interleave2(*gens):
        gens = [g for g in gens if g is not None]
        while gens:
            for g in list(gens):
                try:
                    next(g)
                except StopIteration:
                    gens.remove(g)

    load_ii(0)
    if NT > 1:
        load_ii(1)
    interleave2(Gpart(0))
    for ti in range(NT):
        if ti + 2 < NT:
            load_ii(ti + 2)
        interleave2(Rpart(ti), Gpart(ti + 1) if ti + 1 < NT else None)

    if stage == 2:
        P.emit(nc)
        return nc, st

    barrier()
    PH.reset()
    iota1 = PH.alloc("iota1", [128, NE])
    DMA("sync", iota1[:], iota_d.partition_broadcast(128), [], [iota1.b])
    mk_all = PH.alloc("mk_all", [128, NT, NE], BF16)
    cumx = PH.alloc("cumx", [128, NT + 1, NE], BF16)
    TS("vector", mk_all[:], wts_all[:], 0.0, None, ALU.is_gt, None, [wts_all.b], [mk_all.b])
    MSET("vector", cumx[:, 0, :], 0.0, [cumx.b])
    for ti in range(NT):
        TT("vector", cumx[:, ti + 1, :], cumx[:, ti, :], mk_all[:, ti, :], ALU.add, [cumx.b, mk_all.b], [cumx.b])
    cnt = PH.alloc("cnt", [128, NE])
    pad = PH.alloc("pad", [128, NE])
    pends = PH.alloc("pends", [128, NE])
    pstart = PH.alloc("pstart", [128, NE])
    bk = bank()
    MM(pf32(bk)[:, 0:NE], ones_b[:], cumx[:, NT, :], [ones_b.b, cumx.b], [psb[bk]])
    CP("vector", cnt[:], pf32(bk)[:, 0:NE], [psb[bk]], [cnt.b])
    MSET("vector", pad[:], 0.0, [pad.b])
    for m in range(NT):
        STT("vector", pad[:], cnt[:], float(128 * m), pad[:], ALU.is_gt, ALU.add, [cnt.b, pad.b], [pad.b])
    TS("vector", pad[:], pad[:], 128.0, None, ALU.mult, None, [pad.b], [pad.b])
    P.add("vector", lambda e: e.tensor_tensor_scan(out=pends[:], data0=ones_f[:], data1=pad[:], initial=0.0, op0=ALU.mult, op1=ALU.add),
          reads=[pad.b, ones_f.b], writes=[pends.b])
    TT("vector", pstart[:], pends[:], pad[:], ALU.subtract, [pends.b, pad.b], [pstart.b])
    be = PH.alloc("be", [128, NQ])
    thr = PH.alloc("thr", [128, NQ])
    cmpj = PH.alloc("cmpj", [128, NE])
    Dg = PH.alloc("Dg", [128, NQ, 128])
    for q in range(NQ):
        TS("vector", thr[:, q:q + 1], misc[:, 0:1], 128.0, float(q * 128 * 128), ALU.mult, ALU.add, [misc.b], [thr.b])
        TS("vector", cmpj[:], pends[:], thr[:, q:q + 1], None, ALU.is_le, None, [pends.b, thr.b, cmpj.b], [cmpj.b])
        RSUM("vector", be[:, q:q + 1], cmpj[:], [cmpj.b], [be.b])
        TS("gpsimd", Dg[:, q, :], cst_f[:, 0, :], be[:, q:q + 1], None, ALU.mult, None, [cst_f.b, be.b], [Dg.b])
    NBP = NQ * 128
    ber = PH.alloc("ber", [128, NBP + 1])
    sameb = PH.alloc("sameb", [128, NBP])
    widf = PH.alloc("widf", [128, NBP])
    MSET("vector", ber[:, 0:1], -1.0, [ber.b])
    for q in range(NQ):
        bk = bank()
        MM(pf32(bk)[:, 0:128], ones_f[:, 0:128], Dg[:, q, :], [ones_f.b, Dg.b], [psb[bk]])
        CP("vector", ber[:, 1 + q * 128:1 + (q + 1) * 128], pf32(bk)[:, 0:128], [psb[bk], ber.b], [ber.b])
    TT("vector", sameb[:], ber[:, 1:NBP + 1], ber[:, 0:NBP], ALU.is_equal, [ber.b], [sameb.b])
    TS("vector", widf[:], ber[:, 1:NBP + 1], 128.0, misc[:, 0:1], ALU.mult, ALU.add, [ber.b, misc.b], [widf.b])
    STT("vector", widx[:], sameb[:], 1048576.0, widf[:], ALU.mult, ALU.add, [sameb.b, widf.b], [widx.b])
    dst1 = PH.alloc("dst1", [128, NE])
    km = PH.alloc("km", [128, NE])
    d8f = PH.alloc("d8f", [128, 8])
    e8 = PH.alloc("e8", [128, 8])
    eq8 = PH.alloc("eq8", [128, 8, NE])
    h2l = [PH.alloc("h2l%d" % i, [128, D], BF16) for i in range(2)]
    d8_all.bt = [Buf("d8t") for _ in range(NT)]
    w8_all.bt = [Buf("w8t") for _ in range(NT)]
    DMA("sync", h2l[0][:], h2_d[0:128, :], [h2_db], [h2l[0].b])
    for ti in range(NT):
        if ti + 1 < NT:
            DMA("sync", h2l[(ti + 1) % 2][:], h2_d[(ti + 1) * 128:(ti + 2) * 128, :], [h2_db], [h2l[(ti + 1) % 2].b])
        bk = bank()
        MM(pf32(bk)[:, 0:NE], cst_b[:, 1, :], mk_all[:, ti, :], [cst_b.b, mk_all.b], [psb[bk]], start=True, stop=False)
        MM(pf32(bk)[:, 0:NE], ones_b[:], cumx[:, ti, :], [ones_b.b, cumx.b], [psb[bk]], start=False, stop=True)
        STT("vector", dst1[:], pf32(bk)[:, 0:NE], 1.0, pstart[:], ALU.add, ALU.add, [psb[bk], pstart.b, dst1.b], [dst1.b])
        TT("vector", dst1[:], dst1[:], mk_all[:, ti, :], ALU.mult, [dst1.b, mk_all.b], [dst1.b])
        MAX8(d8f[:], dst1[:], [dst1.b], [d8f.b])
        TS("vector", d8_all[:, ti, :], d8f[:], -1.0, None, ALU.add, None, [d8f.b], [d8_all.bt[ti]])
        TT("vector", km[:], iota1[:], mk_all[:, ti, :], ALU.mult, [iota1.b, mk_all.b, km.b], [km.b])
        MAX8(e8[:], km[:], [km.b], [e8.b])
        for k in range(8):
            STT("vector", eq8[:, k, :], km[:], e8[:, k:k + 1], wts_all[:, ti, :], ALU.is_equal, ALU.mult,
                [km.b, e8.b, wts_all.b, eq8.b], [eq8.b])
        RSUM("vector", w8_all[:, ti, :], eq8[:], [eq8.b], [w8_all.bt[ti]])
        h2t = h2l[ti % 2]
        for k in range(8):
            P.add("gpsimd", (lambda ix, src: lambda e: e.indirect_dma_start(
                out=xs_d, out_offset=bass.IndirectOffsetOnAxis(ap=ix, axis=0), in_=src, in_offset=None))(d8_all[:, ti, k:k + 1], h2t[:]),
                reads=[h2t.b, d8_all.bt[ti]], writes=[Buf("xs_part")], dma=True)

    barrier()
    PH.reset()
    wgb = [PH.alloc("wgb%d" % i, [128, 8, 256], BF16) for i in range(2)]
    wub = [PH.alloc("wub%d" % i, [128, 8, 256], BF16) for i in range(2)]
    wdb = [PH.alloc("wdb%d" % i, [128, 2, D], BF16) for i in range(2)]
    wgf = [PH.alloc("wgf", [128, 2048])] * 2
    wuf = [PH.alloc("wuf", [128, 2048])] * 2
    wdf = [PH.alloc("wdf", [128, 2048])] * 2
    xbl = [PH.alloc("xbl%d" % i, [128, D], BF16) for i in range(2)]
    xbT = [PH.alloc("xbT%d" % i, [128, 8, 128], BF16) for i in range(2)]
    sgl2 = PH.alloc("sgl2", [128, 256])
    actE = PH.alloc("actE", [128, 2, 128], BF16)
    obt = [PH.alloc("obt%d" % i, [128, D]) for i in range(2)]
    _breg = {}

    def bound_reg(e):
        if "r" not in _breg:
            _breg["r"] = e.to_reg(NE * 128 - 1)
        return _breg["r"]

    def e_load(blk):
        DMA("sync", xbl[blk % 2][:], xs_d[blk * 128:(blk + 1) * 128, :], [], [xbl[blk % 2].b])

    def e_gather(blk):
        for wt, src in ((wgf[blk % 2], mg_d), (wuf[blk % 2], mup_d), (wdf[blk % 2], md_d)):
            P.add("gpsimd", (lambda o_, s_, ix: lambda e: e.indirect_dma_start(
                out=o_, out_offset=None, in_=s_, in_offset=bass.IndirectOffsetOnAxis(ap=ix, axis=0),
                bounds_check=bound_reg(e), oob_is_err=False))(wt[:], src, widx[:, blk:blk + 1]),
                reads=[widx.b], writes=[wt.b], dma=True)

    def e_cast(blk):
        wg, wu, wd = wgb[blk % 2], wub[blk % 2], wdb[blk % 2]
        wgs, wus, wds = wgf[blk % 2], wuf[blk % 2], wdf[blk % 2]
        CP("vector", wg[:].rearrange("p a b -> p (a b)"), wgs[:], [wgs.b], [wg.b])
        CP("vector", wu[:].rearrange("p a b -> p (a b)"), wus[:], [wus.b], [wu.b])
        CP("scalar", wd[:, 0, :], wds[:, 0:D], [wds.b], [wd.b])
        CP("scalar", wd[:, 1, :], wds[:, D:2 * D], [wds.b], [wd.b])

    def e_tr_pe(blk):
        bk = bank()
        xb = xbl[blk % 2]
        for kc in range(8):
            TR(pbf(bk)[:, kc * 128:(kc + 1) * 128], xb[:, kc * 128:(kc + 1) * 128], ident_b, [xb.b, cst_b.b], [psb[bk]])
        return bk

    def e_tr_cp(blk, bk):
        CP("scalar", xbT[blk % 2][:].rearrange("p a b -> p (a b)"), pbf(bk)[:, 0:1024], [psb[bk]], [xbT[blk % 2].b])

    e_load(0)
    e_load(1)
    e_gather(0)
    e_cast(0)
    e_gather(1)
    e_tr_cp(0, e_tr_pe(0))
    for blk in range(NBLK):
        wg, wu, wd, ob, xt_ = wgb[blk % 2], wub[blk % 2], wdb[blk % 2], obt[blk % 2], xbT[blk % 2]
        bk = bank()
        for j in range(4):
            wsrc = wg if j < 2 else wu
            for kc in range(8):
                MM(pf32(bk)[:, j * 128:(j + 1) * 128], wsrc[:, kc, (j % 2) * 128:(j % 2 + 1) * 128], xt_[:, kc, :], [wsrc.b, xt_.b],
                   [psb[bk]], start=(kc == 0), stop=(kc == 7))
        bkT = e_tr_pe(blk + 1) if blk + 1 < NBLK else None
        if blk + 1 < NBLK:
            e_cast(blk + 1)
        if blk + 2 < NBLK:
            e_load(blk + 2)
            e_gather(blk + 2)
        ACT(sgl2[:], pf32(bk)[:, 0:256], AF.Silu, [psb[bk]], [sgl2.b])
        TT("vector", actE[:].rearrange("p a b -> p (a b)"), pf32(bk)[:, 256:512], sgl2[:], ALU.mult, [psb[bk], sgl2.b], [actE.b])
        if blk + 1 < NBLK:
            e_tr_cp(blk + 1, bkT)
        for n in range(2):
            bk = bank()
            for fc in range(2):
                MM(pf32(bk), actE[:, fc, :], wd[:, fc, n * 512:(n + 1) * 512], [actE.b, wd.b], [psb[bk]], start=(fc == 0), stop=(fc == 1))
            CP("scalar" if n else "vector", ob[:, n * 512:(n + 1) * 512], pf32(bk), [psb[bk]], [ob.b])
        DMA("sync", ob_d[blk * 128:(blk + 1) * 128, :], ob[:], [ob.b], [Buf("ob_part")])

    barrier()
    NGK = 8
    gk = [PH.alloc("gk%d" % i, [128, D]) for i in range(NGK)]
    accl = [PH.alloc("accl%d" % i, [128, D]) for i in range(2)]
    ssum = PH.alloc("ssum", [128, D])
    sqf3 = PH.alloc("sqf3", [128, D])
    ssn3 = PH.alloc("ssn3", [128, 4])
    outt = [PH.alloc("outt%d" % i, [128, D]) for i in range(2)]
    fing = PH.alloc("fing", [128, D])
    DMA("sync", fing[:], fing_d.partition_broadcast(128), [], [fing.b])
    gi = 0
    DMA("sync", accl[0][:], acc_d[0:128, :], [acc_db], [accl[0].b])
    for ti in range(NT):
        ac = accl[ti % 2]
        if ti + 1 < NT:
            DMA("sync", accl[(ti + 1) % 2][:], acc_d[(ti + 1) * 128:(ti + 2) * 128, :], [acc_db], [accl[(ti + 1) % 2].b])
        for k in range(8):
            g = gk[gi % NGK]
            gi += 1
            P.add("gpsimd", (lambda o_, ix: lambda e: e.indirect_dma_start(
                out=o_, out_offset=None, in_=ob_d, in_offset=bass.IndirectOffsetOnAxis(ap=ix, axis=0)))(g[:], d8_all[:, ti, k:k + 1]),
                reads=[d8_all.bt[ti]], writes=[g.b], dma=True)
            if k == 0:
                TS("vector", ssum[:], g[:], w8_all[:, ti, 0:1], None, ALU.mult, None, [g.b, w8_all.bt[ti], ssum.b], [ssum.b])
            else:
                STT("vector", ssum[:], g[:], w8_all[:, ti, k:k + 1], ssum[:], ALU.mult, ALU.add, [g.b, w8_all.bt[ti], ssum.b], [ssum.b])
        TT("vector", ssum[:], ssum[:], gt2, ALU.mult, [ssum.b, modr.b], [ssum.b])
        TT("vector", ssum[:], ssum[:], ac[:], ALU.add, [ssum.b, ac.b], [ssum.b])
        rmsnorm_rstd(ssum, sqf3, ssn3)
        ot = outt[ti % 2]
        STT("vector", ot[:], ssum[:], ssn3[:, 2:3], fing[:], ALU.mult, ALU.mult, [ssum.b, ssn3.b, fing.b], [ot.b])
        DMA("sync", out_d[ti * 128:(ti + 1) * 128, :], ot[:], [ot.b], [outb])
    P.emit(nc)
    return nc, st


def _consts():
    i = np.arange(128)
    ident = np.eye(128, dtype=np.float32)
    strict = (i[:, None] < i[None, :]).astype(np.float32)
    incl = (i[:, None] <= i[None, :]).astype(np.float32)
    lower = (i[:, None] > i[None, :]).astype(np.float32)
    blk = ((i[:, None] // 64) == (i[None, :] // 64)).astype(np.float32)
    cst = np.stack([ident, strict, incl, lower, blk], axis=1)
    misc = np.zeros((128, 4), np.float32)
    misc[:, 0] = i
    return np.ascontiguousarray(cst), misc


def _shared_maps(inp):
    f = lambda a: np.ascontiguousarray(np.asarray(a, dtype=np.float32))
    cst, misc = _consts()
    fm4 = lambda v: np.asarray(v, np.float32).reshape(4, 128).T
    pf = np.stack([fm4(inp["rw_w0"][0]), fm4(inp["rw_a0"][0]), fm4(inp["rw_k_k"][0]), fm4(inp["rw_k_a"][0]),
                   fm4(inp["rw_r_k"][0].reshape(-1)), fm4(inp["rw_lnx_g"][0]), fm4(inp["rw_lnx_b"][0])], axis=1)
    mg = np.asarray(inp["moe_w_gate"][0], np.float32).reshape(NE, 8, 128, 256).transpose(0, 2, 1, 3).reshape(NE * 128, 2048)
    mu_ = np.asarray(inp["moe_w_up"][0], np.float32).reshape(NE, 8, 128, 256).transpose(0, 2, 1, 3).reshape(NE * 128, 2048)
    md = np.asarray(inp["moe_w_down"][0], np.float32).reshape(NE, 2, 128, D).transpose(0, 2, 1, 3).reshape(NE * 128, 2048)
    m = {
        "w_ada": f(inp["w_ada"][0]), "b_ada": f(inp["b_ada"][0]), "norm1_g": f(inp["norm1_g"][0]),
        "norm2_g": f(inp["norm2_g"][0]), "final_g": f(inp["final_g"]), "w_in": f(inp["w_in"][0]),
        "mu_fm": f(np.asarray(inp["rw_mu"][0], np.float32).reshape(14, 128).T), "pf": f(pf),
        "rw_w2": f(inp["rw_w2"][0]), "rw_a2": f(inp["rw_a2"][0]), "rw_g2": f(inp["rw_g2"][0]),
        "gm_ln_g": f(np.asarray(inp["gm_ln_g"][0]).reshape(-1)), "gm_ln_b": f(np.asarray(inp["gm_ln_b"][0]).reshape(-1)),
        "wsT": f(np.asarray(inp["gm_w_s"][0], np.float32).transpose(2, 0, 1)), "bs_tm": f(np.asarray(inp["gm_b_s"][0]).T),
        "w_br_rwkv": f(inp["w_br_rwkv"][0]), "w_br_gmlp": f(inp["w_br_gmlp"][0]), "w_out": f(inp["w_out"][0]),
        "router_w": f(inp["router_w"][0]), "router_b": f(inp["router_b"][0]),
        "moe_wg": np.ascontiguousarray(mg), "moe_wu": np.ascontiguousarray(mu_), "moe_wd": np.ascontiguousarray(md),
        "sh_w_gate": f(inp["sh_w_gate"][0]), "sh_w_up": f(inp["sh_w_up"][0]), "sh_w_down": f(inp["sh_w_down"][0]),
        "cst": cst, "misc": misc, "iota1": np.arange(1, NE + 1, dtype=np.float32),
    }
    return m


def _core_map(shared, inp, b, S):
    m = dict(shared)
    m["x"] = np.ascontiguousarray(np.asarray(inp["x"][b, :S], np.float32))
    m["c_pk"] = np.ascontiguousarray(np.asarray(inp["c"][b], np.float32).reshape(8, 128).T)
    return m


def kernel(**inputs):
    B = inputs["x"].shape[0]
    S = inputs["x"].shape[1]
    nc, st = build(S)
    shared = _shared_maps(inputs)
    in_maps = [_core_map(shared, inputs, b, S) for b in range(B)]
    res = run_bass_kernel_spmd(nc, in_maps, core_ids=list(range(B)))
    st.close()
    return np.stack([r["out"] for r in res.results], axis=0).astype(np.float32)
```
